# Optimizing a Trainium2 kernel written in Bass

```python
import jax, jax.numpy as jnp
from jax import lax
import numpy as np

D_MODEL = 1024
BATCH = 4
SEQ = 8192
DEPTH = 2

N_EVEN = (DEPTH + 1) // 2
N_ODD = DEPTH // 2

POOL_WINDOWS = (2, 4, 8, 16)
POOL_GROUPS = len(POOL_WINDOWS)
POOL_GROUP_DIM = D_MODEL // POOL_GROUPS

SSM_EXPAND = 2
SSM_D_INNER = SSM_EXPAND * D_MODEL
SSM_HEAD_DIM = 64
SSM_N_HEADS = SSM_D_INNER // SSM_HEAD_DIM
SSM_N_GROUPS = 8
SSM_HEADS_PER_GROUP = SSM_N_HEADS // SSM_N_GROUPS
SSM_D_STATE = 128
SSM_CONV = 4
SSM_CHUNK = 128
SSM_BC_DIM = SSM_N_GROUPS * SSM_D_STATE
SSM_CONV_DIM = SSM_D_INNER + 2 * SSM_BC_DIM
SSM_IN_DIM = SSM_D_INNER + SSM_CONV_DIM + SSM_N_HEADS

D_FF_DENSE = 2816
N_EXPERTS = 8
TOP_K = 2
D_FF_EXPERT = 3584

NORM_EPS = 1e-6
SSM_NORM_EPS = 1e-5
DT_MIN = 1e-3
DT_MAX = 1e-1

kernel_name = "hybrid_pool_ssd_moe_trunk"


def rmsnorm(x, w, eps=NORM_EPS):
    xf = x.astype(jnp.float32)
    y = xf * lax.rsqrt(jnp.mean(xf * xf, axis=-1, keepdims=True) + eps)
    return (y * w.astype(jnp.float32)).astype(x.dtype)


def swiglu(t, w_gate, w_up, w_down):
    return jnp.matmul(jax.nn.silu(jnp.matmul(t, w_gate)) * jnp.matmul(t, w_up), w_down)


def pool_mixer(h, w_grp, scale):
    b, l, d = h.shape
    hf = h.astype(jnp.float32).reshape(b, l, POOL_GROUPS, POOL_GROUP_DIM)
    cs0 = jnp.pad(jnp.cumsum(hf, axis=1), ((0, 0), (1, 0), (0, 0), (0, 0)))
    pos = jnp.arange(l)
    outs = []
    for g, w in enumerate(POOL_WINDOWS):
        upper = cs0[:, 1:, g]
        lower = jnp.pad(cs0[:, :l + 1 - w, g], ((0, 0), (w - 1, 0), (0, 0)))
        count = jnp.minimum(pos + 1, w).astype(jnp.float32)[None, :, None]
        outs.append((upper - lower) / count - hf[:, :, g])
    p = jnp.stack(outs, axis=2)
    y = jnp.einsum('blgc,gce->blge', p, w_grp.astype(jnp.float32)).reshape(b, l, d)
    return (y * scale.astype(jnp.float32)).astype(h.dtype)


def ssd_scan(xs, dt, a, bm, cm):
    b, l = xs.shape[:2]
    nc = l // SSM_CHUNK

    def to_chunks(t):
        return jnp.moveaxis(t.reshape(b, nc, SSM_CHUNK, *t.shape[2:]), 1, 0)

    causal = jnp.tril(jnp.ones((SSM_CHUNK, SSM_CHUNK), dtype=bool))[None, :, :, None, None]

    def step(state, inp):
        x_c, dt_c, b_c, c_c = inp
        acs = jnp.cumsum(dt_c * a, axis=1)
        seg = acs[:, :, None] - acs[:, None, :]
        decay = jnp.exp(jnp.where(causal, seg, -jnp.inf))
        cb = jnp.einsum('bign,bjgn->bijg', c_c, b_c)
        w = cb[..., None] * decay * dt_c[:, None]
        y_diag = jnp.einsum('bijgr,bjgrp->bigrp', w, x_c)
        y_off = jnp.einsum('bign,bgrpn->bigrp', c_c, state) * jnp.exp(acs)[..., None]
        to_end = jnp.exp(acs[:, -1:] - acs) * dt_c
        new_state = (state * jnp.exp(acs[:, -1])[..., None, None]
                     + jnp.einsum('bjgn,bjgr,bjgrp->bgrpn', b_c, to_end, x_c))
        return new_state, y_diag + y_off

    state0 = jnp.zeros((b, SSM_N_GROUPS, SSM_HEADS_PER_GROUP, SSM_HEAD_DIM, SSM_D_STATE), jnp.float32)
    _, y = lax.scan(step, state0, (to_chunks(xs), to_chunks(dt), to_chunks(bm), to_chunks(cm)))
    return jnp.moveaxis(y, 0, 1).reshape(xs.shape)


def ssd_mixer(h, w_in, conv_w, conv_b, dt_bias, a_log, d_skip, gate_norm_w, w_out):
    b, l, _ = h.shape
    f32 = jnp.float32
    zxbcdt = jnp.einsum('bld,de->ble', h, w_in).astype(f32)
    z = zxbcdt[..., :SSM_D_INNER]
    xbc = zxbcdt[..., SSM_D_INNER:SSM_D_INNER + SSM_CONV_DIM]
    dt = zxbcdt[..., SSM_D_INNER + SSM_CONV_DIM:]
    xbc = lax.conv_general_dilated(
        xbc, conv_w.astype(f32)[:, None, :], window_strides=(1,),
        padding=[(SSM_CONV - 1, 0)], dimension_numbers=('NWC', 'WIO', 'NWC'),
        feature_group_count=SSM_CONV_DIM) + conv_b.astype(f32)
    xbc = jax.nn.silu(xbc)
    xs = xbc[..., :SSM_D_INNER].reshape(b, l, SSM_N_GROUPS, SSM_HEADS_PER_GROUP, SSM_HEAD_DIM)
    bm = xbc[..., SSM_D_INNER:SSM_D_INNER + SSM_BC_DIM].reshape(b, l, SSM_N_GROUPS, SSM_D_STATE)
    cm = xbc[..., SSM_D_INNER + SSM_BC_DIM:].reshape(b, l, SSM_N_GROUPS, SSM_D_STATE)
    dt = jax.nn.softplus(dt + dt_bias.astype(f32)).reshape(b, l, SSM_N_GROUPS, SSM_HEADS_PER_GROUP)
    a = -jnp.exp(a_log.astype(f32)).reshape(SSM_N_GROUPS, SSM_HEADS_PER_GROUP)
    y = ssd_scan(xs, dt, a, bm, cm)
    y = y + d_skip.astype(f32).reshape(SSM_N_GROUPS, SSM_HEADS_PER_GROUP)[..., None] * xs
    y = y.reshape(b, l, SSM_D_INNER)
    y = rmsnorm(y * jax.nn.silu(z), gate_norm_w, SSM_NORM_EPS)
    return jnp.einsum('ble,ed->bld', y.astype(h.dtype), w_out).astype(h.dtype)


def moe_swiglu(h, w_router, w_gate, w_up, w_down):
    b, l, d = h.shape
    t = h.reshape(b * l, d)
    logits = jnp.matmul(t, w_router).astype(jnp.float32)
    top_val, top_idx = lax.top_k(logits, TOP_K)
    gates = jax.nn.softmax(top_val, axis=-1)
    combine = jnp.sum(jax.nn.one_hot(top_idx, N_EXPERTS, dtype=jnp.float32) * gates[..., None], axis=1)
    out = jnp.zeros((b * l, d), jnp.float32)
    for e in range(N_EXPERTS):
        y_e = swiglu(t, w_gate[e], w_up[e], w_down[e]).astype(jnp.float32)
        out = out + combine[:, e:e + 1] * y_e
    return out.reshape(b, l, d).astype(h.dtype)


def setup_inputs(seed: int = 0) -> dict:
    key = jax.random.key(seed)
    ks = jax.random.split(key, 32)
    f32 = jnp.float32

    def nrm(k, shape, fan_in):
        return jax.random.normal(k, shape, f32) * (fan_in ** -0.5)

    def gain(k, shape):
        return 1.0 + 0.1 * jax.random.normal(k, shape, f32)

    dt0 = jnp.exp(jax.random.uniform(ks[14], (N_ODD, SSM_N_HEADS), f32)
                  * (np.log(DT_MAX) - np.log(DT_MIN)) + np.log(DT_MIN))
    dt_bias = dt0 + jnp.log(-jnp.expm1(-dt0))
    return {
        "x": jax.random.normal(ks[0], (BATCH, SEQ, D_MODEL), f32),
        "pool_norm_w": gain(ks[1], (N_EVEN, D_MODEL)),
        "pool_w": nrm(ks[2], (N_EVEN, POOL_GROUPS, POOL_GROUP_DIM, POOL_GROUP_DIM), POOL_GROUP_DIM),
        "pool_scale": 0.5 + 0.05 * jax.random.normal(ks[3], (N_EVEN, D_MODEL), f32),
        "dense_norm_w": gain(ks[4], (N_EVEN, D_MODEL)),
        "dense_w_gate": nrm(ks[5], (N_EVEN, D_MODEL, D_FF_DENSE), D_MODEL),
        "dense_w_up": nrm(ks[6], (N_EVEN, D_MODEL, D_FF_DENSE), D_MODEL),
        "dense_w_down": nrm(ks[7], (N_EVEN, D_FF_DENSE, D_MODEL), D_FF_DENSE),
        "ssd_norm_w": gain(ks[8], (N_ODD, D_MODEL)),
        "ssd_w_in": nrm(ks[9], (N_ODD, D_MODEL, SSM_IN_DIM), D_MODEL),
        "ssd_conv_w": 0.5 * jax.random.normal(ks[10], (N_ODD, SSM_CONV, SSM_CONV_DIM), f32),
        "ssd_conv_b": 0.02 * jax.random.normal(ks[11], (N_ODD, SSM_CONV_DIM), f32),
        "ssd_dt_bias": dt_bias,
        "ssd_a_log": jnp.log(jax.random.uniform(ks[12], (N_ODD, SSM_N_HEADS), f32, 1.0, 16.0)),
        "ssd_d": gain(ks[13], (N_ODD, SSM_N_HEADS)),
        "ssd_gate_norm_w": gain(ks[15], (N_ODD, SSM_D_INNER)),
        "ssd_w_out": nrm(ks[16], (N_ODD, SSM_D_INNER, D_MODEL), SSM_D_INNER),
        "moe_norm_w": gain(ks[17], (N_ODD, D_MODEL)),
        "moe_w_router": nrm(ks[18], (N_ODD, D_MODEL, N_EXPERTS), D_MODEL),
        "moe_w_gate": nrm(ks[19], (N_ODD, N_EXPERTS, D_MODEL, D_FF_EXPERT), D_MODEL),
        "moe_w_up": nrm(ks[20], (N_ODD, N_EXPERTS, D_MODEL, D_FF_EXPERT), D_MODEL),
        "moe_w_down": nrm(ks[21], (N_ODD, N_EXPERTS, D_FF_EXPERT, D_MODEL), D_FF_EXPERT),
        "final_norm_w": gain(ks[22], (D_MODEL,)),
    }


def reference(x, pool_norm_w, pool_w, pool_scale, dense_norm_w, dense_w_gate, dense_w_up,
              dense_w_down, ssd_norm_w, ssd_w_in, ssd_conv_w, ssd_conv_b, ssd_dt_bias,
              ssd_a_log, ssd_d, ssd_gate_norm_w, ssd_w_out, moe_norm_w, moe_w_router,
              moe_w_gate, moe_w_up, moe_w_down, final_norm_w):
    for i in range(DEPTH):
        j = i // 2
        if i % 2 == 0:
            x = x + pool_mixer(rmsnorm(x, pool_norm_w[j]), pool_w[j], pool_scale[j])
            x = x + swiglu(rmsnorm(x, dense_norm_w[j]), dense_w_gate[j], dense_w_up[j],
                           dense_w_down[j]).astype(x.dtype)
        else:
            x = x + ssd_mixer(rmsnorm(x, ssd_norm_w[j]), ssd_w_in[j], ssd_conv_w[j], ssd_conv_b[j],
                              ssd_dt_bias[j], ssd_a_log[j], ssd_d[j], ssd_gate_norm_w[j], ssd_w_out[j])
            x = x + moe_swiglu(rmsnorm(x, moe_norm_w[j]), moe_w_router[j], moe_w_gate[j],
                               moe_w_up[j], moe_w_down[j])
    return rmsnorm(x, final_norm_w)
```

```python
import numpy as np
import concourse.bass as bass
import concourse.mybir as mybir
from concourse.bass_utils import run_bass_kernel_spmd

F32 = mybir.dt.float32
BF16 = mybir.dt.bfloat16
AF = mybir.ActivationFunctionType
ALU = mybir.AluOpType

P = 128
D = 1024
KD = 8
DFF = 2816
DFE = 3584
NE = 8
DIN = 2048
NH = 32
HD = 64
NG = 8
NTOK = 4096
NPRE = 4096
HALF = 2048
POOL_WINDOWS = (2, 4, 8, 16)
EPS = 1e-6
SSM_EPS = 1e-5

C_DNW, C_SNW, C_MNW, C_CW, C_CB, C_GNW, C_FLAG = 0, 8, 16, 24, 152, 184, 200
NCOLS = 201
R_PNW, R_FNW, R_PSC = 0, 1024, 2048
R_DTB, R_ALOG, R_D = 0, 32, 64
NREPS = 96


class Tk:
    __slots__ = ("name", "w", "r")

    def __init__(self, name=""):
        self.name = name
        self.w = None
        self.r = {}


class Slot:
    def __init__(self, S, name):
        self.key = "dma_" + name
        self.sem = S.nc.alloc_semaphore(name="q_" + name)
        self.cnt = 0


class Sched:
    def __init__(self, nc):
        self.nc = nc
        self.engs = {}
        for name, e in [("pe", nc.tensor), ("act", nc.scalar), ("dve", nc.vector),
                        ("pool", nc.gpsimd), ("sp", nc.sync)]:
            self.engs[name] = dict(e=e, key=name, sem=nc.alloc_semaphore(name="s_" + name),
                                   cnt=0, waited={})
        self.n_ops = 0
        self.n_wait = 0

    def slot(self, name):
        return Slot(self, name)

    def _wait_deps(self, E, reads, writes):
        deps = {}

        def add(d):
            if d is None:
                return
            k = d[0]
            if k not in deps or deps[k][2] < d[2]:
                deps[k] = d
        for t in reads:
            add(t.w)
        for t in writes:
            add(t.w)
            for d in t.r.values():
                add(d)
        for k, (key, sem, val) in deps.items():
            if key == E["key"] and key == "pe":
                continue
            if E["waited"].get(key, 0) >= val:
                continue
            if key == E["key"]:
                assert val <= E["cnt"], "own-engine dep on pending instruction"
            E["e"].wait_ge(sem, val)
            E["waited"][key] = val
            self.n_wait += 1

    def _mark(self, my, reads, writes):
        for t in reads:
            old = t.r.get(my[0])
            if old is None or old[2] < my[2]:
                t.r[my[0]] = my
        for t in writes:
            t.w = my
            t.r = {}

    def op(self, eng, fn, reads=(), writes=(), inc=True):
        E = self.engs[eng]
        self._wait_deps(E, reads, writes)
        ins = fn(E["e"])
        self.n_ops += 1
        if inc:
            E["cnt"] += 1
            ins.then_inc(E["sem"], 1)
            my = (E["key"], E["sem"], E["cnt"])
        else:
            my = (E["key"], E["sem"], E["cnt"] + 1)
        self._mark(my, reads, writes)
        return ins

    def dma(self, eng, slot, pairs, reads=(), writes=()):
        E = self.engs[eng]
        self._wait_deps(E, reads, writes)
        for (o, i) in pairs:
            ins = E["e"].dma_start(out=o, in_=i)
            slot.cnt += 16
            ins.then_inc(slot.sem, 16)
            self.n_ops += 1
        my = (slot.key, slot.sem, slot.cnt)
        self._mark(my, reads, writes)

    def wait_all(self, eng, tks):
        self._wait_deps(self.engs[eng], [], tks)


class Buf:
    def __init__(self, t, name):
        self.t = t
        self.k = Tk(name)


class Ring:
    def __init__(self, bufs):
        self.bufs = bufs
        self.i = 0

    def next(self):
        b = self.bufs[self.i % len(self.bufs)]
        self.i += 1
        return b


class Builder:
    def __init__(self, stop=None):
        flags = (stop or "").split(",")
        self.small = "small" in flags
        self.skip = set(flags)
        self.nolite = "nolite" in flags
        self.nofull = "nofull" in flags
        stop = flags[0] if flags[0] else None
        self.stop = stop
        nc = self.nc = bass.Bass("TRN2", target_bir_lowering=False)
        self.S = Sched(nc)
        self.ctx = []
        dt = nc.dram_tensor

        def inp(name, shape):
            return dt(name, list(shape), F32, kind="ExternalInput").ap()
        self.c_sq = inp("c_sq", [5, P, P])
        self.c_cols = inp("c_cols", [P, NCOLS])
        self.c_reps = inp("c_reps", [P, NREPS])
        self.c_big = inp("c_big", [P, 3 * D])
        self.w_r = inp("moe_w_router", [D, NE])
        if stop == "moe2":
            self.xs = inp("xs", [NTOK, D])
        else:
            self.xs = inp("xs", [NPRE + NTOK, D])
            self.c_band = inp("c_band", [4, 4, P, P])
            self.pool_w = inp("pool_w", [4, 256, 256])
            self.w_dg = inp("dense_w_gate", [D, DFF])
            self.w_du = inp("dense_w_up", [D, DFF])
            self.w_dd = inp("dense_w_down", [DFF, D])
            self.w_in = inp("ssd_w_in", [D, 6176])
            self.w_out = inp("ssd_w_out", [DIN, D])
            self.w_in_b = dt("w_in_b", [24, P, KD * 256], BF16).ap()
            self.w_out_b = dt("w_out_b", [KD, P, 16 * P], BF16).ap()
            self.winb_k = [Tk(f"winb{t}") for t in range(24)]
            self.woutb_k = [Tk(f"woutb{t}") for t in range(KD)]
        if stop is None or stop in ("nopre", "moe2"):
            self.w_mg = inp("moe_w_gate", [NE, D, DFE])
            self.w_mu = inp("moe_w_up", [NE, D, DFE])
            self.w_md = inp("moe_w_down", [NE, DFE, D])
        self.out = dt("out", [NTOK, D], F32, kind="ExternalOutput").ap()

    def slot(self, name):
        if name not in self._slots:
            self._slots[name] = self.S.slot(name)
        return self._slots[name]

    def xtk(self, ks, c0, n):
        return [self.xk[k][b] for k in ks for b in range(c0 // P, (c0 + n + P - 1) // P)]

    def sb(self, name, shape, dtype):
        self.uid = getattr(self, "uid", 0) + 1
        name = f"{name}_{self.uid}"
        g = self.nc.sbuf_tensor(name, list(shape), dtype)
        t = g.__enter__()
        self.ctx.append(g)
        return Buf(t, name)

    def ps(self, name, shape, dtype=F32):
        g = self.nc.psum_tensor(name, list(shape), dtype)
        t = g.__enter__()
        self.ctx.append(g)
        return Buf(t, name)

    def mark(self):
        return len(self.ctx)

    def release(self, m):
        while len(self.ctx) > m:
            self.ctx.pop().__exit__(None, None, None)

    def build(self):
        S = self.S
        self._slots = {}
        self.q_w = [self.slot("w0"), self.slot("w1")]
        self.q_w2 = [self.slot("v0"), self.slot("v1")]
        self.q_wo = [self.slot("o0"), self.slot("o1")]
        self.wctr = 0
        self.xk = [[Tk(f"xT{k}_{b}") for b in range(HALF // P)] for k in range(KD)]
        if self.stop == "moe2":
            return self.build_moe2()
        self.setup()
        self.PS = self.ps("psum", [P, 8, 512])
        self.B = [Buf(self.PS.t[:, i, :], f"bank{i}") for i in range(8)]
        self.xT = self.sb("xT", [P, KD, HALF], F32)
        self.htok = [self.sb(f"htok{i}", [P, D], BF16) for i in range(3)]
        self.hidx = 0
        self.halo = self.sb("halo", [P, 32, 3], F32)
        self.S_f = self.sb("S_f", [P, DIN], F32)
        self.S_b = self.sb("S_b", [P, DIN], BF16)
        S.op("pool", lambda e: e.memset(self.halo.t[:], 0.0), writes=[self.halo.k])
        S.op("pool", lambda e: e.memset(self.S_f.t[:], 0.0), writes=[self.S_f.k])
        S.op("pool", lambda e: e.memset(self.htok[2].t[:], 0.0), writes=[self.htok[2].k])
        self.hprev = self.htok[2]

        if self.stop != "nopre":
            ngrp = NPRE // 1024
            for grp in range(1 if self.small else ngrp):
                r0 = grp * 1024
                self.pool_phase(r0, 1024, first=("pre0" if grp == 0 else None))
                if self.stop == "pool":
                    continue
                self.ffn_dense(1024)
                if self.stop == "ffn":
                    continue
                for sg in range(2):
                    if self.nolite:
                        continue
                    self.ssd_group(sg * 512, 512, lite=True, with_c=(grp == ngrp - 1 and sg == 1))
        S.op("dve", lambda e: e.tensor_scalar(self.S_f.t[:], self.S_f.t[:], self.cols.t[:, C_FLAG:C_FLAG + 1], None,
                                              op0=ALU.mult), reads=[self.S_f.k, self.cols.k], writes=[self.S_f.k])
        S.op("act", lambda e: e.copy(self.S_b.t[:], self.S_f.t[:]), reads=[self.S_f.k], writes=[self.S_b.k])

        for half in range(1 if self.small else 2):
            r0 = NPRE + half * HALF
            self.pool_phase(r0, HALF, first=("main0" if half == 0 else None))
            if self.stop == "pool":
                self.final_phase(half, norm=False)
                continue
            for tg in range(1 if self.small else HALF // 1024):
                self.ffn_dense(1024, c0=tg * 1024)
            if self.stop == "ffn":
                self.final_phase(half, norm=False)
                continue
            for sg in range(1 if self.small else HALF // 256):
                if self.nofull:
                    continue
                self.ssd_group(sg * 256, 256, lite=False)
            if self.stop == "ssd":
                self.final_phase(half, norm=False)
                continue
            self.moe_half()
            self.final_phase(half, norm=("mn" not in self.skip))
        if not self.small:
            S.wait_all("sp", [self.otk[0], self.otk[1]])
        self.release(0)
        return self.nc

    def build_moe2(self):
        S = self.S
        self.csq = self.sb("csq", [P, 5, P], F32)
        self.ident_f = self.csq.t[:, 0, :]
        self.tri_f = self.csq.t[:, 1, :]
        self.lmat_f = self.csq.t[:, 2, :]
        self.ones_f = self.csq.t[:, 3, :]
        self.onesmean_f = self.csq.t[:, 4, :]
        self.cols = self.sb("cols", [P, NCOLS], F32)
        self.wr = self.sb("wr", [P, KD, NE], F32)
        S.dma("sp", self.slot("c1"), [(self.csq.t[:], self.c_sq.rearrange("c p q -> p c q")),
                                      (self.cols.t[:], self.c_cols),
                                      (self.wr.t[:], self.w_r.rearrange("(k p) e -> p k e", p=P))],
              writes=[self.csq.k, self.cols.k, self.wr.k])
        self.PS = self.ps("psum", [P, 8, 512])
        self.B = [Buf(self.PS.t[:, i, :], f"bank{i}") for i in range(8)]
        self.xT = self.sb("xT", [P, KD, HALF], F32)
        for half in range(2):
            self.load_phase(half)
            self.moe_half()
            self.final_phase(half, norm=True)
        S.wait_all("sp", [self.otk[0], self.otk[1]])
        self.release(0)
        return self.nc

    def load_phase(self, half):
        S, B = self.S, self.B
        m = self.mark()
        xtok = Ring([self.sb(f"xtok{i}", [P, D], F32) for i in range(2)])
        for ti in range(HALF // P):
            xt = xtok.next()
            c0 = ti * P
            r = half * HALF + c0
            S.dma("sp", self.slot("x%d" % (ti % 2)), [(xt.t[:], self.xs[r:r + P, :])], writes=[xt.k])
            for k in range(KD):
                bk = B[k // 4]
                S.op("pe", lambda e: e.transpose(bk.t[:, (k % 4) * P:(k % 4 + 1) * P], xt.t[:, k * P:(k + 1) * P], self.ident_f),
                     reads=[xt.k, self.csq.k], writes=[bk.k], inc=(k % 4 == 3))
            for hb in range(2):
                S.op("act", lambda e: e.copy(self.xT.t[:, hb * 4:(hb + 1) * 4, c0:c0 + P],
                                             B[hb].t[:].rearrange("p (k t) -> p k t", k=4)),
                     reads=[B[hb].k], writes=self.xtk(range(hb * 4, hb * 4 + 4), c0, P))
        self.release_after(m, [xtok.bufs[0].k, xtok.bufs[1].k])

    def setup(self):
        S, nc = self.S, self.nc
        self.csq = self.sb("csq", [P, 5, P], F32)
        self.ident_f = self.csq.t[:, 0, :]
        self.tri_f = self.csq.t[:, 1, :]
        self.lmat_f = self.csq.t[:, 2, :]
        self.ones_f = self.csq.t[:, 3, :]
        self.onesmean_f = self.csq.t[:, 4, :]
        self.cols = self.sb("cols", [P, NCOLS], F32)
        self.reps = self.sb("reps", [P, NREPS], F32)
        self.identb = self.sb("identb", [P, P], BF16)
        self.bands = self.sb("bands", [P, 4, 4, P], BF16)
        self.poolw = self.sb("poolw", [P, 4, 2, 256], BF16)
        self.wdt = self.sb("wdt", [P, KD, NH], BF16)
        self.wr = self.sb("wr", [P, KD, NE], F32)
        self.aneg = self.sb("aneg", [P, NH], F32)
        S.dma("sp", self.slot("c1"), [(self.csq.t[:], self.c_sq.rearrange("c p q -> p c q")),
                               (self.cols.t[:], self.c_cols), (self.reps.t[:], self.c_reps),
                               (self.wr.t[:], self.w_r.rearrange("(k p) e -> p k e", p=P))],
              writes=[self.csq.k, self.cols.k, self.reps.k, self.wr.k])
        S.dma("pool", self.slot("c2"), [(self.bands.t[:], self.c_band.rearrange("a g p q -> p a g q")),
                                 (self.wdt.t[:], self.w_in[:, 6144:6176].rearrange("(k p) n -> p k n", p=P))],
              writes=[self.bands.k, self.wdt.k])
        S.op("dve", lambda e: e.tensor_copy(self.identb.t[:], self.ident_f), reads=[self.csq.k], writes=[self.identb.k])
        S.op("act", lambda e: e.activation(self.aneg.t[:], self.reps.t[:, R_ALOG:R_ALOG + NH], AF.Exp),
             reads=[self.reps.k], writes=[self.aneg.k])
        S.op("dve", lambda e: e.tensor_scalar(self.aneg.t[:], self.aneg.t[:], -1.0, None, op0=ALU.mult),
             reads=[self.aneg.k], writes=[self.aneg.k])
        m = self.mark()
        pw32 = self.sb("pw32", [P, 4, 2, 256], F32)
        pscb = self.sb("pscb", [P, D], F32)
        S.dma("sp", self.slot("c3"), [(pw32.t[:], self.pool_w.rearrange("g (kc p) e -> p g kc e", p=P)),
                                      (pscb.t[:], self.c_big[:, R_PSC:R_PSC + D])], writes=[pw32.k, pscb.k])
        psc = pscb.t[:].rearrange("p (g e) -> p g e", g=4)
        for kc in range(2):
            S.op("dve", lambda e: e.tensor_tensor(self.poolw.t[:, :, kc, :], pw32.t[:, :, kc, :], psc, ALU.mult),
                 reads=[pw32.k, pscb.k], writes=[self.poolw.k])
        self.release_after(m, [pw32.k, pscb.k])
        m = self.mark()
        stg = Ring([self.sb(f"stg{i}", [P, KD * 256], BF16) for i in range(4)])
        for t in range(24 + KD):
            st_ = stg.next()
            if t < 24:
                src = self.w_in[:, t * 256:(t + 1) * 256].rearrange("(k p) n -> p k n", p=P)
                dst, dk_ = self.w_in_b[t], self.winb_k[t]
                sview = st_.t[:].rearrange("p (k n) -> p k n", k=KD)
            else:
                dq = t - 24
                src = self.w_out[:, dq * P:(dq + 1) * P].rearrange("(j p) d -> p j d", p=P)
                dst, dk_ = self.w_out_b[dq], self.woutb_k[dq]
                sview = st_.t[:].rearrange("p (j d) -> p j d", j=16)
            S.dma("pool", self.slot("pc%d" % (t % 4)), [(sview, src)], writes=[st_.k])
            S.dma("sp", self.slot("pd%d" % (t % 4)), [(dst, st_.t[:])], reads=[st_.k], writes=[dk_])
        self.release_after(m, [b_.k for b_ in stg.bufs])

    def pool_phase(self, r0, ntok, first):
        S = self.S
        m = self.mark()
        xtok = Ring([self.sb(f"xtok{i}", [P, D], F32) for i in range(2)])
        junk = self.sb("pjunk", [P, D], BF16)
        pT = self.sb("pT", [P, KD, P], BF16)
        st = Ring([self.sb(f"pst{i}", [P, 4], F32) for i in range(2)])
        pnwb = self.sb("pnwb", [P, D], F32)
        S.dma("sp", self.slot("c4"), [(pnwb.t[:], self.c_big[:, R_PNW:R_PNW + D])], writes=[pnwb.k])
        pnw = pnwb.t[:]
        B = self.B
        for ti in range(ntok // P):
            xt = xtok.next()
            s4 = st.next()
            c0 = ti * P
            S.dma("sp", self.slot("x%d" % (ti % 2)), [(xt.t[:], self.xs[r0 + c0:r0 + c0 + P, :])], writes=[xt.k])
            S.op("act", lambda e: e.activation(junk.t[:], xt.t[:], AF.Square, accum_out=s4.t[:, 0:1]),
                 reads=[xt.k], writes=[junk.k, s4.k])
            S.op("act", lambda e: e.activation(s4.t[:, 1:2], s4.t[:, 0:1], AF.Sqrt, bias=EPS, scale=1.0 / D),
                 reads=[s4.k], writes=[s4.k])
            S.op("dve", lambda e: e.reciprocal(s4.t[:, 2:3], s4.t[:, 1:2]), reads=[s4.k], writes=[s4.k])
            hc = self.htok[self.hidx % 2]
            self.hidx += 1
            S.op("dve", lambda e: e.scalar_tensor_tensor(hc.t[:], xt.t[:], s4.t[:, 2:3], pnw, op0=ALU.mult, op1=ALU.mult),
                 reads=[xt.k, s4.k, pnwb.k], writes=[hc.k])
            for k in range(KD):
                bk = B[k // 4]
                S.op("pe", lambda e: e.transpose(bk.t[:, (k % 4) * P:(k % 4 + 1) * P], xt.t[:, k * P:(k + 1) * P], self.ident_f),
                     reads=[xt.k, self.csq.k], writes=[bk.k], inc=(k % 4 == 3))
            for hb in range(2):
                S.op("act", lambda e: e.copy(self.xT.t[:, hb * 4:(hb + 1) * 4, c0:c0 + P],
                                             B[hb].t[:].rearrange("p (k t) -> p k t", k=4)),
                     reads=[B[hb].k], writes=self.xtk(range(hb * 4, hb * 4 + 4), c0, P))
            kind_cur = 0
            if ti == 0 and first == "pre0":
                kind_cur = 2
            if ti == 0 and first == "main0":
                kind_cur = 3
            hp = self.hprev
            for k in range(KD):
                g = k // 2
                bk = B[2 + k // 4]
                o = bk.t[:, (k % 4) * P:(k % 4 + 1) * P]
                S.op("pe", lambda e: e.matmul(o, lhsT=hp.t[:, k * P:(k + 1) * P], rhs=self.bands.t[:, 1, g, :], start=True, stop=False),
                     reads=[hp.k, self.bands.k], writes=[bk.k], inc=False)
                S.op("pe", lambda e: e.matmul(o, lhsT=hc.t[:, k * P:(k + 1) * P], rhs=self.bands.t[:, kind_cur, g, :], start=False, stop=True),
                     reads=[hc.k, self.bands.k], writes=[bk.k], inc=(k % 4 == 3))
            for hb in range(2):
                S.op("act", lambda e: e.copy(pT.t[:, hb * 4:(hb + 1) * 4, :], B[2 + hb].t[:].rearrange("p (k t) -> p k t", k=4)),
                     reads=[B[2 + hb].k], writes=[pT.k])
            for k in range(KD):
                g, ec = k // 2, k % 2
                bk = B[4 + k // 4]
                o = bk.t[:, (k % 4) * P:(k % 4 + 1) * P]
                for kc in range(2):
                    S.op("pe", lambda e: e.matmul(o, lhsT=self.poolw.t[:, g, kc, ec * P:(ec + 1) * P], rhs=pT.t[:, 2 * g + kc, :],
                                                  start=(kc == 0), stop=(kc == 1)),
                         reads=[self.poolw.k, pT.k], writes=[bk.k], inc=(k % 4 == 3 and kc == 1))
            for hb in range(2):
                xs_ = self.xT.t[:, hb * 4:(hb + 1) * 4, c0:c0 + P]
                S.op("dve", lambda e: e.tensor_tensor(xs_, B[4 + hb].t[:].rearrange("p (k t) -> p k t", k=4), xs_, ALU.add),
                     reads=[B[4 + hb].k] + self.xtk(range(hb * 4, hb * 4 + 4), c0, P), writes=self.xtk(range(hb * 4, hb * 4 + 4), c0, P))
            self.hprev = hc
        self.release_after(m, [xtok.bufs[0].k, xtok.bufs[1].k, junk.k, pT.k, st.bufs[0].k, st.bufs[1].k, pnwb.k])

    def release_after(self, m, tks):
        for eng in ("pe", "act", "dve", "pool", "sp"):
            self.S.wait_all(eng, tks)
        self.release(m)

    def norm_fm(self, c0, n, nwcol, hn, hn_c0, scratch, eps=EPS):
        S = self.S
        sq, sd, rs = scratch
        bst = self.B[6]
        for k in range(KD):
            q = sq.next()
            S.op("act", lambda e: e.activation(q.t[:, :n], self.xT.t[:, k, c0:c0 + n], AF.Square),
                 reads=self.xtk([k], c0, n), writes=[q.k])
            S.op("pe", lambda e: e.matmul(bst.t[:, :n], lhsT=self.onesmean_f, rhs=q.t[:, :n], start=(k == 0), stop=(k == KD - 1)),
                 reads=[q.k, self.csq.k], writes=[bst.k])
        S.op("act", lambda e: e.activation(sd.t[:, :n], bst.t[:, :n], AF.Sqrt, bias=eps, scale=1.0),
             reads=[bst.k], writes=[sd.k])
        S.op("dve", lambda e: e.reciprocal(rs.t[:, :n], sd.t[:, :n]), reads=[sd.k], writes=[rs.k])
        for k in range(KD):
            S.op("dve", lambda e: e.scalar_tensor_tensor(hn.t[:, k, hn_c0:hn_c0 + n], self.xT.t[:, k, c0:c0 + n],
                                                         self.cols.t[:, nwcol + k:nwcol + k + 1], rs.t[:, :n],
                                                         op0=ALU.mult, op1=ALU.mult),
                 reads=self.xtk([k], c0, n) + [self.cols.k, rs.k], writes=[hn.k])

    def norm_scratch(self, w=512):
        sq = Ring([self.sb(f"nsq{i}", [P, w], F32) for i in range(2)])
        sd = self.sb("nsd", [P, w], F32)
        rs = self.sb("nrs", [P, w], F32)
        return (sq, sd, rs), [sq.bufs[0].k, sq.bufs[1].k, sd.k, rs.k]

    def ffn_bufs(self):
        wg = [self.sb(f"wg{i}", [P, KD, 512], BF16) for i in range(2)]
        wu = [self.sb(f"wu{i}", [P, KD, 512], BF16) for i in range(2)]
        wd = [self.sb(f"wd{i}", [P, 4, D], BF16) for i in range(2)]
        sg = Ring([self.sb(f"sg{i}", [P, 2, 512], F32) for i in range(2)])
        act = Ring([self.sb(f"act{i}", [P, 4, 512], BF16) for i in range(2)])
        tks = [b.k for b in wg + wu + wd + sg.bufs + act.bufs]
        return dict(wg=wg, wu=wu, wd=wd, sg=sg, act=act), tks

    def ffn_pass(self, fb, hn, ntok, c0, g_ap, u_ap, d_ap, F, comb=None):
        S, B, PS = self.S, self.B, self.PS
        f0 = 0
        pending = None
        while f0 < F:
            W = min(512, F - f0)
            ncx = W // P
            b = self.wctr % 2
            self.wctr += 1
            wg, wu, wd = fb["wg"][b], fb["wu"][b], fb["wd"][b]
            S.dma("pool", self.q_w[b],
                  [(wg.t[:, :, :W], g_ap[:, f0:f0 + W].rearrange("(k p) n -> p k n", p=P)),
                   (wu.t[:, :, :W], u_ap[:, f0:f0 + W].rearrange("(k p) n -> p k n", p=P)),
                   (wd.t[:, :ncx, :], d_ap[f0:f0 + W, :].rearrange("(c p) d -> p c d", p=P))],
                  writes=[wg.k, wu.k, wd.k])
            for s0 in range(0, ntok, 512):
                n = min(512, ntok - s0)
                a = fb["act"].next()
                for cp in range(ncx // 2):
                    for (w, bb) in ((wg, 0), (wu, 2)):
                        for c2 in range(2):
                            c = 2 * cp + c2
                            pb = B[bb + c2]
                            for k in range(KD):
                                S.op("pe", lambda e: e.matmul(pb.t[:, :n], lhsT=w.t[:, k, c * P:(c + 1) * P], rhs=hn.t[:, k, s0:s0 + n],
                                                              start=(k == 0), stop=(k == KD - 1)),
                                     reads=[w.k, hn.k], writes=[pb.k], inc=(k == KD - 1))
                    sg = fb["sg"].next()
                    S.op("act", lambda e: e.activation(sg.t[:, :, :n], PS.t[:, 0:2, :n], AF.Silu),
                         reads=[B[0].k, B[1].k], writes=[sg.k])
                    if comb is None:
                        S.op("dve", lambda e: e.tensor_tensor(a.t[:, 2 * cp:2 * cp + 2, :n], sg.t[:, :, :n], PS.t[:, 2:4, :n], ALU.mult),
                             reads=[sg.k, B[2].k, B[3].k], writes=[a.k])
                    else:
                        S.op("dve", lambda e: e.tensor_tensor(sg.t[:, :, :n], sg.t[:, :, :n], PS.t[:, 2:4, :n], ALU.mult),
                             reads=[sg.k, B[2].k, B[3].k], writes=[sg.k])
                        S.op("dve", lambda e: e.tensor_tensor(a.t[:, 2 * cp:2 * cp + 2, :n], sg.t[:, :, :n],
                                                              comb.t[:, s0:s0 + n].unsqueeze(1).to_broadcast([P, 2, n]), ALU.mult),
                             reads=[sg.k, comb.k], writes=[a.k])
                if pending is not None:
                    pending()
                pending = self._down_closure(a, wd, ncx, n, c0 + s0)
            f0 += W
        if pending is not None:
            pending()

    def _down_closure(self, a, wd, ncx, n, col0):
        S, B, PS = self.S, self.B, self.PS

        def run():
            for dp in range(4):
                ob = 4 + 2 * (dp % 2)
                for d2 in range(2):
                    dk = 2 * dp + d2
                    po = B[ob + d2]
                    for c in range(ncx):
                        S.op("pe", lambda e: e.matmul(po.t[:, :n], lhsT=wd.t[:, c, dk * P:(dk + 1) * P], rhs=a.t[:, c, :n],
                                                      start=(c == 0), stop=(c == ncx - 1)),
                             reads=[wd.k, a.k], writes=[po.k], inc=(c == ncx - 1))
                xs_ = self.xT.t[:, 2 * dp:2 * dp + 2, col0:col0 + n]
                xk_ = self.xtk([2 * dp, 2 * dp + 1], col0, n)
                S.op("dve", lambda e: e.tensor_tensor(xs_, PS.t[:, ob:ob + 2, :n], xs_, ALU.add),
                     reads=[B[ob].k, B[ob + 1].k] + xk_, writes=xk_)
        return run

    def ffn_dense(self, ntok, c0=0):
        m = self.mark()
        hn = self.sb("hn_d", [P, KD, ntok], BF16)
        scr, stk = self.norm_scratch()
        fb, ftk = self.ffn_bufs()
        for s0 in range(0, ntok, 512):
            self.norm_fm(c0 + s0, 512, C_DNW, hn, s0, scr)
        self.ffn_pass(fb, hn, ntok, c0, self.w_dg, self.w_du, self.w_dd, DFF)
        self.release_after(m, [hn.k] + stk + ftk)

    def moe_half(self):
        S, B = self.S, self.B
        m = self.mark()
        hn = self.sb("hn_m", [P, KD, HALF], BF16)
        comb_tok = self.sb("comb_tok", [P, HALF // P, NE], F32)
        comb = self.sb("comb_rep", [P, HALF], F32)
        m2 = self.mark()
        scr, stk = self.norm_scratch(512)
        sq, sd, rs = scr
        hf = self.sb("hf", [P, KD, 512], F32)
        sm = Ring([self.sb(f"rsm{i}", [P, 48], F32) for i in range(2)])
        bst, blg = B[6], B[7]
        for s0 in range(0, HALF, 512):
            n = 512
            for k in range(KD):
                q = sq.next()
                S.op("act", lambda e: e.activation(q.t[:, :n], self.xT.t[:, k, s0:s0 + n], AF.Square),
                     reads=self.xtk([k], s0, n), writes=[q.k])
                S.op("pe", lambda e: e.matmul(bst.t[:, :n], lhsT=self.onesmean_f, rhs=q.t[:, :n], start=(k == 0), stop=(k == KD - 1)),
                     reads=[q.k, self.csq.k], writes=[bst.k])
            S.op("act", lambda e: e.activation(sd.t[:, :n], bst.t[:, :n], AF.Sqrt, bias=EPS, scale=1.0), reads=[bst.k], writes=[sd.k])
            S.op("dve", lambda e: e.reciprocal(rs.t[:, :n], sd.t[:, :n]), reads=[sd.k], writes=[rs.k])
            for k in range(KD):
                S.op("dve", lambda e: e.scalar_tensor_tensor(hf.t[:, k, :], self.xT.t[:, k, s0:s0 + n],
                                                             self.cols.t[:, C_MNW + k:C_MNW + k + 1], rs.t[:, :n],
                                                             op0=ALU.mult, op1=ALU.mult),
                     reads=self.xtk([k], s0, n) + [self.cols.k, rs.k], writes=[hf.k])
            S.op("act", lambda e: e.copy(hn.t[:, :, s0:s0 + n], hf.t[:]), reads=[hf.k], writes=[hn.k])
            for tt in range(4):
                ti = s0 // P + tt
                for k in range(KD):
                    S.op("pe", lambda e: e.matmul(blg.t[:, 0:NE], lhsT=hf.t[:, k, tt * P:(tt + 1) * P], rhs=self.wr.t[:, k, :],
                                                  start=(k == 0), stop=(k == KD - 1)),
                         reads=[hf.k, self.wr.k], writes=[blg.k], inc=(k == KD - 1))
                r = sm.next()
                lg, mx, nm1, sel, ex, se, den = (r.t[:, 0:8], r.t[:, 8:16], r.t[:, 16:17], r.t[:, 24:32], r.t[:, 32:40],
                                                 r.t[:, 40:48], r.t[:, 17:18])
                S.op("act", lambda e: e.copy(lg, blg.t[:, 0:NE]), reads=[blg.k], writes=[r.k])
                S.op("dve", lambda e: e.max(mx, lg), reads=[r.k], writes=[r.k])
                S.op("dve", lambda e: e.tensor_scalar(nm1, mx[:, 0:1], -1.0, None, op0=ALU.mult), reads=[r.k], writes=[r.k])
                S.op("dve", lambda e: e.tensor_scalar(sel, lg, mx[:, 1:2], None, op0=ALU.is_ge), reads=[r.k], writes=[r.k])
                S.op("act", lambda e: e.activation(ex, lg, AF.Exp, bias=nm1, scale=1.0), reads=[r.k], writes=[r.k])
                S.op("dve", lambda e: e.tensor_tensor(se, sel, ex, ALU.mult), reads=[r.k], writes=[r.k])
                S.op("dve", lambda e: e.reduce_sum(den, se, mybir.AxisListType.X), reads=[r.k], writes=[r.k])
                S.op("dve", lambda e: e.reciprocal(r.t[:, 18:19], den), reads=[r.k], writes=[r.k])
                S.op("dve", lambda e: e.tensor_scalar(comb_tok.t[:, ti, :], se, r.t[:, 18:19], None, op0=ALU.mult),
                     reads=[r.k], writes=[comb_tok.k])
        self.release_after(m2, stk + [hf.k, sm.bufs[0].k, sm.bufs[1].k])
        fb, ftk = self.ffn_bufs()
        dg = Ring([self.sb(f"dg{i}", [P, P], F32) for i in range(2)])
        elist = list(range(NE))
        if "me1" in self.skip:
            elist = [0]
        if "meA" in self.skip:
            elist = [7]
        if "meB" in self.skip:
            elist = [0, 1, 2, 3]
        if "meC" in self.skip:
            elist = [0, 1, 2, 3, 4, 5]
        for ex_ in elist:
            if "mc" in self.skip:
                continue
            for tq in range(HALF // 512):
                bk = B[6 + tq % 2]
                for t4 in range(4):
                    ti = tq * 4 + t4
                    d_ = dg.next()
                    S.op("dve", lambda e: e.tensor_scalar(d_.t[:], self.ident_f, comb_tok.t[:, ti, ex_:ex_ + 1], None, op0=ALU.mult),
                         reads=[self.csq.k, comb_tok.k], writes=[d_.k])
                    S.op("pe", lambda e: e.matmul(bk.t[:, t4 * P:(t4 + 1) * P], lhsT=self.ones_f, rhs=d_.t[:], start=True, stop=True),
                         reads=[d_.k, self.csq.k], writes=[bk.k])
                S.op("act", lambda e: e.copy(comb.t[:, tq * 512:(tq + 1) * 512], bk.t[:]), reads=[bk.k], writes=[comb.k])
            if "mx" in self.skip:
                continue
            self.ffn_pass(fb, hn, HALF, 0, self.w_mg[ex_], self.w_mu[ex_], self.w_md[ex_], DFE, comb=comb)
        self.release_after(m, [hn.k, comb_tok.k, comb.k, dg.bufs[0].k, dg.bufs[1].k] + ftk)

    def final_phase(self, half, norm=True):
        S, B = self.S, self.B
        m = self.mark()
        ot = Ring([self.sb(f"otile{i}", [P, D], F32) for i in range(2)])
        junk = self.sb("fjunk", [P, D], BF16)
        st = Ring([self.sb(f"fst{i}", [P, 4], F32) for i in range(2)])
        fnwb = self.sb("fnwb", [P, D], F32)
        S.dma("sp", self.slot("c5"), [(fnwb.t[:], self.c_big[:, R_FNW:R_FNW + D])], writes=[fnwb.k])
        fnw = fnwb.t[:]
        self.otk = [ot.bufs[0].k, ot.bufs[1].k]
        for ti in range(HALF // P):
            c0 = ti * P
            o = ot.next()
            s4 = st.next()
            for k in range(KD):
                bk = B[k // 4]
                S.op("pe", lambda e: e.transpose(bk.t[:, (k % 4) * P:(k % 4 + 1) * P], self.xT.t[:, k, c0:c0 + P], self.ident_f),
                     reads=self.xtk([k], c0, P) + [self.csq.k], writes=[bk.k], inc=(k % 4 == 3))
            if norm:
                for hb in range(2):
                    S.op("act", lambda e: e.activation(junk.t[:, hb * 512:(hb + 1) * 512], B[hb].t[:], AF.Square,
                                                       accum_out=s4.t[:, hb:hb + 1]),
                         reads=[B[hb].k], writes=[junk.k, s4.k])
                S.op("dve", lambda e: e.tensor_tensor(s4.t[:, 2:3], s4.t[:, 0:1], s4.t[:, 1:2], ALU.add), reads=[s4.k], writes=[s4.k])
                S.op("act", lambda e: e.activation(s4.t[:, 3:4], s4.t[:, 2:3], AF.Sqrt, bias=EPS, scale=1.0 / D), reads=[s4.k], writes=[s4.k])
                S.op("dve", lambda e: e.reciprocal(s4.t[:, 0:1], s4.t[:, 3:4]), reads=[s4.k], writes=[s4.k])
                for hb in range(2):
                    S.op("dve", lambda e: e.scalar_tensor_tensor(o.t[:, hb * 512:(hb + 1) * 512], B[hb].t[:], s4.t[:, 0:1],
                                                                 fnw[:, hb * 512:(hb + 1) * 512], op0=ALU.mult, op1=ALU.mult),
                         reads=[B[hb].k, s4.k, fnwb.k], writes=[o.k])
            else:
                for hb in range(2):
                    S.op("act", lambda e: e.copy(o.t[:, hb * 512:(hb + 1) * 512], B[hb].t[:]), reads=[B[hb].k], writes=[o.k])
            r = half * HALF + c0
            S.dma("sp", self.slot("out%d" % (ti % 2)), [(self.out[r:r + P, :], o.t[:])], reads=[o.k])
        self.release_after(m, [ot.bufs[0].k, ot.bufs[1].k, junk.k, st.bufs[0].k, st.bufs[1].k, fnwb.k])

    def ssd_group(self, c0, ntok, lite, with_c=True):
        S, B = self.S, self.B
        nch = ntok // P
        m = self.mark()
        hn = self.sb("hn_s", [P, KD, ntok], BF16)
        m1 = self.mark()
        scr, stk = self.norm_scratch(256)
        for s0 in range(0, ntok, 256):
            self.norm_fm(c0 + s0, 256, C_SNW, hn, s0, scr)
        self.release_after(m1, stk)
        win = [self.sb(f"win{i}", [P, KD, 256], BF16) for i in range(2)]
        nxc = 32 if (with_c or not lite) else 24
        if "fc" in self.skip:
            nxc = 24
        xbcT = self.sb("xbcT", [P, nxc, ntok], BF16)
        cs = Ring([self.sb(f"cs{i}", [P, ntok + 3], F32) for i in range(2)])
        ca = Ring([self.sb(f"ca{i}", [P, ntok], F32) for i in range(2)])
        tks = [hn.k, xbcT.k] + [b.k for b in win + cs.bufs + ca.bufs]
        if not lite:
            zs = self.sb("zs", [P, nch, DIN], BF16)
            tks.append(zs.k)
            for cg in range(0 if "fz" in self.skip else DIN // 256):
                b = self.wctr % 2
                self.wctr += 1
                w = win[b]
                S.dma("sp", self.q_w2[b], [(w.t[:], self.w_in_b[cg].rearrange("p (k n) -> p k n", k=KD))],
                      reads=[self.winb_k[cg]], writes=[w.k])
                for ch in range(nch):
                    pb = B[ch % 2]
                    for k in range(KD):
                        S.op("pe", lambda e: e.matmul(pb.t[:, :256], lhsT=hn.t[:, k, ch * P:(ch + 1) * P], rhs=w.t[:, k, :],
                                                      start=(k == 0), stop=(k == KD - 1)),
                             reads=[hn.k, w.k], writes=[pb.k], inc=(k == KD - 1))
                    S.op("act", lambda e: e.activation(zs.t[:, ch, cg * 256:(cg + 1) * 256], pb.t[:, :256], AF.Silu),
                         reads=[pb.k], writes=[zs.k])
        wcur = [None]

        def xbc_stage1(j):
            cg, j2 = j // 2, j % 2
            if j2 == 0:
                b = self.wctr % 2
                self.wctr += 1
                wcur[0] = win[b]
                S.dma("sp", self.q_w2[b], [(wcur[0].t[:], self.w_in_b[8 + cg].rearrange("p (k n) -> p k n", k=KD))],
                      reads=[self.winb_k[8 + cg]], writes=[wcur[0].k])
            w = wcur[0]
            cb_ = cs.next()
            S.op("act", lambda e: e.copy(cb_.t[:, 0:3], self.halo.t[:, j, :]), reads=[self.halo.k], writes=[cb_.k])
            for s0 in range(0, ntok, 512):
                n = min(512, ntok - s0)
                pb = B[(j2 + s0 // 512) % 2]
                for k in range(KD):
                    S.op("pe", lambda e: e.matmul(pb.t[:, :n], lhsT=w.t[:, k, j2 * P:(j2 + 1) * P], rhs=hn.t[:, k, s0:s0 + n],
                                                  start=(k == 0), stop=(k == KD - 1)),
                         reads=[hn.k, w.k], writes=[pb.k], inc=(k == KD - 1))
                S.op("act", lambda e: e.copy(cb_.t[:, 3 + s0:3 + s0 + n], pb.t[:, :n]), reads=[pb.k], writes=[cb_.k])
            S.op("act", lambda e: e.copy(self.halo.t[:, j, :], cb_.t[:, ntok:ntok + 3]), reads=[cb_.k], writes=[self.halo.k])
            return cb_

        def xbc_stage2(j, cb_):
            a_ = ca.next()
            cw = self.cols.t[:, C_CW + 4 * j:C_CW + 4 * j + 4]
            S.op("dve", lambda e: e.tensor_scalar(a_.t[:], cb_.t[:, 0:ntok], cw[:, 0:1], self.cols.t[:, C_CB + j:C_CB + j + 1],
                                                  op0=ALU.mult, op1=ALU.add),
                 reads=[cb_.k, self.cols.k], writes=[a_.k])
            for tap in range(1, 4):
                S.op("dve", lambda e: e.scalar_tensor_tensor(a_.t[:], cb_.t[:, tap:tap + ntok], cw[:, tap:tap + 1], a_.t[:],
                                                             op0=ALU.mult, op1=ALU.add),
                     reads=[cb_.k, self.cols.k, a_.k], writes=[a_.k])
            S.op("act", lambda e: e.activation(xbcT.t[:, j, :], a_.t[:], AF.Silu), reads=[a_.k], writes=[xbcT.k])

        cb_next = xbc_stage1(0)
        for j in range(nxc):
            cb_cur = cb_next
            if j + 1 < nxc:
                cb_next = xbc_stage1(j + 1)
            xbc_stage2(j, cb_cur)
        dts = self.sb("dts", [P, nch, 6, NH], F32)
        tks.append(dts.k)
        for ch in range(nch):
            pb = B[6]
            for k in range(KD):
                S.op("pe", lambda e: e.matmul(pb.t[:, 0:NH], lhsT=hn.t[:, k, ch * P:(ch + 1) * P], rhs=self.wdt.t[:, k, :],
                                              start=(k == 0), stop=(k == KD - 1)),
                     reads=[hn.k, self.wdt.k], writes=[pb.k], inc=(k == KD - 1))
            xr, ax, ex, ln, dtv, dA = [dts.t[:, ch, i, :] for i in range(6)]
            S.op("dve", lambda e: e.tensor_tensor(xr, pb.t[:, 0:NH], self.reps.t[:, R_DTB:R_DTB + NH], ALU.add),
                 reads=[pb.k, self.reps.k], writes=[dts.k])
            S.op("dve", lambda e: e.scalar_tensor_tensor(ax, xr, -1.0, xr, op0=ALU.mult, op1=ALU.max), reads=[dts.k], writes=[dts.k])
            S.op("act", lambda e: e.activation(ex, ax, AF.Exp, scale=-1.0), reads=[dts.k], writes=[dts.k])
            S.op("act", lambda e: e.activation(ln, ex, AF.Ln, bias=1.0), reads=[dts.k], writes=[dts.k])
            S.op("dve", lambda e: e.scalar_tensor_tensor(dtv, xr, 0.0, ln, op0=ALU.max, op1=ALU.add), reads=[dts.k], writes=[dts.k])
            S.op("dve", lambda e: e.tensor_tensor(dA, dtv, self.aneg.t[:], ALU.mult), reads=[dts.k, self.aneg.k], writes=[dts.k])
        xdt = self.sb("xdt", [P, DIN], BF16)
        xw = self.sb("xw", [P, DIN], BF16)
        btok = self.sb("btok", [P, NG * P], BF16)
        sm = self.sb("ssm", [P, 6, NH], F32)
        tks += [xdt.k, xw.k, btok.k, sm.k]
        if not lite:
            xtok = self.sb("xtk", [P, DIN], BF16)
            R = Ring([self.sb(f"R{i}", [P, 4, P], F32) for i in range(4)])
            dec = Ring([self.sb(f"dec{i}", [P, 512], F32) for i in range(2)])
            wT = Ring([self.sb(f"wT{i}", [P, 4, P], BF16) for i in range(2)])
            cbm = self.sb("cbm", [P, NG, P], F32)
            yh = self.sb("yh", [P, 1024], F32)
            yg = self.sb("yg", [P, DIN], F32)
            yn = self.sb("yn", [P, DIN], BF16)
            ynT = self.sb("ynT", [P, 16, ntok], BF16)
            gst = self.sb("gst", [P, 8], F32)
            tks += [xtok.k, cbm.k, yh.k, yg.k, yn.k, ynT.k, gst.k] + [b.k for b in R.bufs + dec.bufs + wT.bufs]
        for ch in range(nch):
            tc = ch * P
            dtv, dA = dts.t[:, ch, 4, :], dts.t[:, ch, 5, :]
            acs, dif, te, eacs, etot = [sm.t[:, i, :] for i in range(5)]
            ptx = [B[2].t[:].bitcast(BF16), B[3].t[:].bitcast(BF16)]
            for j in range(16):
                S.op("pe", lambda e: e.transpose(ptx[j // 8][:, (j % 8) * P:(j % 8 + 1) * P], xbcT.t[:, j, tc:tc + P], self.identb.t[:]),
                     reads=[xbcT.k, self.identb.k], writes=[B[2 + j // 8].k], inc=(j % 8 == 7))
            for hb in range(2):
                src = ptx[hb].rearrange("p (h q) -> p h q", q=HD)
                S.op("dve", lambda e: e.tensor_tensor(xdt.t[:, hb * 1024:(hb + 1) * 1024].rearrange("p (h q) -> p h q", q=HD), src,
                                                      dtv[:, hb * 16:(hb + 1) * 16].unsqueeze(2).to_broadcast([P, 16, HD]), ALU.mult),
                     reads=[B[2 + hb].k, dts.k], writes=[xdt.k])
                if not lite and "fx" not in self.skip:
                    S.op("act", lambda e: e.copy(xtok.t[:, hb * 1024:(hb + 1) * 1024], ptx[hb]), reads=[B[2 + hb].k], writes=[xtok.k])
            ptb = B[4].t[:].bitcast(BF16)
            for g in range(NG):
                S.op("pe", lambda e: e.transpose(ptb[:, g * P:(g + 1) * P], xbcT.t[:, 16 + g, tc:tc + P], self.identb.t[:]),
                     reads=[xbcT.k, self.identb.k], writes=[B[4].k], inc=(g == NG - 1))
            S.op("act", lambda e: e.copy(btok.t[:], ptb), reads=[B[4].k], writes=[btok.k])
            pa = B[5]
            S.op("pe", lambda e: e.matmul(pa.t[:, 0:NH], lhsT=self.tri_f, rhs=dA, start=True, stop=True),
                 reads=[dts.k, self.csq.k], writes=[pa.k], inc=False)
            S.op("pe", lambda e: e.matmul(pa.t[:, NH:2 * NH], lhsT=self.ones_f, rhs=dA, start=True, stop=True),
                 reads=[dts.k, self.csq.k], writes=[pa.k])
            S.op("act", lambda e: e.copy(acs, pa.t[:, 0:NH]), reads=[pa.k], writes=[sm.k])
            S.op("dve", lambda e: e.tensor_tensor(dif, pa.t[:, NH:2 * NH], acs, ALU.subtract), reads=[pa.k, sm.k], writes=[sm.k])
            S.op("act", lambda e: e.activation(te, dif, AF.Exp), reads=[sm.k], writes=[sm.k])
            S.op("act", lambda e: e.activation(etot, pa.t[:, NH:2 * NH], AF.Exp), reads=[pa.k], writes=[sm.k])
            if not lite and "fe" not in self.skip:
                S.op("act", lambda e: e.activation(eacs, acs, AF.Exp), reads=[sm.k], writes=[sm.k])
            if not lite and "f1" not in self.skip:
                for g in range(NG):
                    bk = B[2 + g // 4]
                    S.op("pe", lambda e: e.matmul(bk.t[:, (g % 4) * P:(g % 4 + 1) * P], lhsT=xbcT.t[:, 16 + g, tc:tc + P],
                                                  rhs=xbcT.t[:, 24 + g, tc:tc + P], start=True, stop=True),
                         reads=[xbcT.k], writes=[bk.k], inc=(g % 4 == 3))
                for hb in range(2):
                    S.op("dve", lambda e: e.tensor_tensor(cbm.t[:, hb * 4:(hb + 1) * 4, :], B[2 + hb].t[:].rearrange("p (g i) -> p g i", g=4),
                                                          self.tri_f.unsqueeze(1).to_broadcast([P, 4, P]), ALU.mult),
                         reads=[B[2 + hb].k, self.csq.k], writes=[cbm.k])
                def stage_a(hq):
                    r_ = R.next()
                    S.op("pool", lambda e: e.tensor_tensor(r_.t[:], self.tri_f.unsqueeze(1).to_broadcast([P, 4, P]),
                                                           dA[:, 4 * hq:4 * hq + 4].unsqueeze(2).to_broadcast([P, 4, P]), ALU.mult),
                         reads=[self.csq.k, dts.k], writes=[r_.k])
                    pseg = B[hq % 2]
                    S.op("pe", lambda e: e.matmul(pseg.t[:], lhsT=self.lmat_f, rhs=r_.t[:].rearrange("p h i -> p (h i)"), start=True, stop=True),
                         reads=[r_.k, self.csq.k], writes=[pseg.k])
                    d_ = dec.next()
                    S.op("act", lambda e: e.activation(d_.t[:], pseg.t[:], AF.Exp), reads=[pseg.k], writes=[d_.k])
                    w_ = wT.next()
                    S.op("dve", lambda e: e.tensor_tensor(w_.t[:], d_.t[:].rearrange("p (h i) -> p h i", h=4),
                                                          cbm.t[:, hq, :].unsqueeze(1).to_broadcast([P, 4, P]), ALU.mult),
                         reads=[d_.k, cbm.k], writes=[w_.k])
                    return w_
                pyd = [B[6], B[7]]
                pyo = [B[2], B[3]]

                def a_pool(hq):
                    r_ = R.next()
                    S.op("pool", lambda e: e.tensor_tensor(r_.t[:], self.tri_f.unsqueeze(1).to_broadcast([P, 4, P]),
                                                           dA[:, 4 * hq:4 * hq + 4].unsqueeze(2).to_broadcast([P, 4, P]), ALU.mult),
                         reads=[self.csq.k, dts.k], writes=[r_.k])
                    return r_

                def a_rest(hq, r_):
                    pseg = B[hq % 2]
                    S.op("pe", lambda e: e.matmul(pseg.t[:], lhsT=self.lmat_f, rhs=r_.t[:].rearrange("p h i -> p (h i)"), start=True, stop=True),
                         reads=[r_.k, self.csq.k], writes=[pseg.k])
                    d_ = dec.next()
                    S.op("act", lambda e: e.activation(d_.t[:], pseg.t[:], AF.Exp), reads=[pseg.k], writes=[d_.k])
                    w_ = wT.next()
                    S.op("dve", lambda e: e.tensor_tensor(w_.t[:], d_.t[:].rearrange("p (h i) -> p h i", h=4),
                                                          cbm.t[:, hq, :].unsqueeze(1).to_broadcast([P, 4, P]), ALU.mult),
                         reads=[d_.k, cbm.k], writes=[w_.k])
                    return w_

                def skip_term():
                    for hh in range(2):
                        cs_ = slice(hh * 1024, (hh + 1) * 1024)
                        S.op("pool", lambda e: e.tensor_tensor(yg.t[:, cs_].rearrange("p (h q) -> p h q", q=HD),
                                                               xtok.t[:, cs_].rearrange("p (h q) -> p h q", q=HD),
                                                               self.reps.t[:, R_D + 16 * hh:R_D + 16 * hh + 16].unsqueeze(2).to_broadcast([P, 16, HD]), ALU.mult),
                             reads=[xtok.k, self.reps.k], writes=[yg.k])

                def tail_dve(hh):
                    for g in range(4 * hh, 4 * hh + 4):
                        gl = g - 4 * hh
                        bk = pyo[gl // 2]
                        S.op("pe", lambda e: e.matmul(bk.t[:, (gl % 2) * 256:(gl % 2 + 1) * 256], lhsT=xbcT.t[:, 24 + g, tc:tc + P],
                                                      rhs=self.S_b.t[:, g * 256:(g + 1) * 256], start=True, stop=True),
                             reads=[xbcT.k, self.S_b.k], writes=[bk.k], inc=(gl % 2 == 1))
                    for q in range(2):
                        hs = slice(hh * 16 + q * 8, hh * 16 + q * 8 + 8)
                        ysl = yh.t[:, q * 512:(q + 1) * 512]
                        S.op("dve", lambda e: e.tensor_tensor(ysl.rearrange("p (h q) -> p h q", q=HD), pyo[q].t[:].rearrange("p (h q) -> p h q", q=HD),
                                                              eacs[:, hs].unsqueeze(2).to_broadcast([P, 8, HD]), ALU.mult),
                             reads=[pyo[q].k, sm.k], writes=[yh.k])
                        S.op("dve", lambda e: e.tensor_tensor(ysl, ysl, pyd[q].t[:], ALU.add), reads=[pyd[q].k, yh.k], writes=[yh.k])
                    cs_ = slice(hh * 1024, (hh + 1) * 1024)
                    S.op("dve", lambda e: e.tensor_tensor(yg.t[:, cs_], yg.t[:, cs_], yh.t[:], ALU.add), reads=[yg.k, yh.k], writes=[yg.k])

                def tail_pool(hh):
                    cs_ = slice(hh * 1024, (hh + 1) * 1024)
                    S.op("pool", lambda e: e.tensor_tensor(yg.t[:, cs_], yg.t[:, cs_], zs.t[:, ch, cs_], ALU.mult),
                         reads=[yg.k, zs.k], writes=[yg.k])

                def tail_sq(hh):
                    cs_ = slice(hh * 1024, (hh + 1) * 1024)
                    S.op("act", lambda e: e.activation(yn.t[:, cs_], yg.t[:, cs_], AF.Square, accum_out=gst.t[:, hh:hh + 1]),
                         reads=[yg.k], writes=[yn.k, gst.k])

                rr = {0: a_pool(0)}
                w_next = a_rest(0, rr[0])
                for hq in range(8):
                    hh = hq // 4
                    w_ = w_next
                    if hq + 1 < 8:
                        if hq + 1 not in rr:
                            rr[hq + 1] = a_pool(hq + 1)
                        w_next = a_rest(hq + 1, rr[hq + 1])
                    if hq == 3:
                        skip_term()
                    for r4 in range(4):
                        h = 4 * hq + r4
                        hl = h - 16 * hh
                        bk = pyd[hl // 8]
                        S.op("pe", lambda e: e.matmul(bk.t[:, (hl % 8) * HD:(hl % 8 + 1) * HD], lhsT=w_.t[:, r4, :],
                                                      rhs=xdt.t[:, h * HD:(h + 1) * HD], start=True, stop=True),
                             reads=[w_.k, xdt.k], writes=[bk.k], inc=(r4 == 3))
                    if hq == 3:
                        tail_dve(0)
                        for q_ in (5, 6, 7):
                            rr[q_] = a_pool(q_)
                        tail_pool(0)
                    if hq == 7:
                        tail_dve(1)
                        tail_pool(1)
                        tail_sq(0)
                        tail_sq(1)
            if not lite and "f2" not in self.skip:
                S.op("dve", lambda e: e.tensor_tensor(gst.t[:, 2:3], gst.t[:, 0:1], gst.t[:, 1:2], ALU.add), reads=[gst.k], writes=[gst.k])
                S.op("act", lambda e: e.activation(gst.t[:, 3:4], gst.t[:, 2:3], AF.Sqrt, bias=SSM_EPS, scale=1.0 / DIN), reads=[gst.k], writes=[gst.k])
                S.op("dve", lambda e: e.reciprocal(gst.t[:, 4:5], gst.t[:, 3:4]), reads=[gst.k], writes=[gst.k])
                S.op("dve", lambda e: e.tensor_scalar(yn.t[:], yg.t[:], gst.t[:, 4:5], None, op0=ALU.mult), reads=[yg.k, gst.k], writes=[yn.k])
                pyt = [B[2].t[:].bitcast(BF16), B[3].t[:].bitcast(BF16)]
                for j in range(16):
                    S.op("pe", lambda e: e.transpose(pyt[j // 8][:, (j % 8) * P:(j % 8 + 1) * P], yn.t[:, j * P:(j + 1) * P], self.identb.t[:]),
                         reads=[yn.k, self.identb.k], writes=[B[2 + j // 8].k], inc=(j % 8 == 7))
                for hb in range(2):
                    S.op("dve", lambda e: e.tensor_tensor(ynT.t[:, hb * 8:(hb + 1) * 8, tc:tc + P], pyt[hb].rearrange("p (j t) -> p j t", j=8),
                                                          self.cols.t[:, C_GNW + hb * 8:C_GNW + hb * 8 + 8].unsqueeze(2).to_broadcast([P, 8, P]), ALU.mult),
                         reads=[B[2 + hb].k, self.cols.k], writes=[ynT.k])
            S.op("pool", lambda e: e.tensor_tensor(xw.t[:].rearrange("p (h q) -> p h q", q=HD),
                                                   xdt.t[:].rearrange("p (h q) -> p h q", q=HD),
                                                   te.unsqueeze(2).to_broadcast([P, NH, HD]), ALU.mult),
                 reads=[xdt.k, sm.k], writes=[xw.k])
            for hh in range(2):
                pst = [B[6], B[7]]
                for g in range(4 * hh, 4 * hh + 4):
                    gl = g - 4 * hh
                    bk = pst[gl // 2]
                    S.op("pe", lambda e: e.matmul(bk.t[:, (gl % 2) * 256:(gl % 2 + 1) * 256], lhsT=btok.t[:, g * P:(g + 1) * P],
                                                  rhs=xw.t[:, g * 256:(g + 1) * 256], start=True, stop=True),
                         reads=[btok.k, xw.k], writes=[bk.k], inc=(gl % 2 == 1))
                cs_ = slice(hh * 1024, (hh + 1) * 1024)
                sf = self.S_f.t[:, cs_]
                S.op("pool", lambda e: e.tensor_tensor(sf.rearrange("p (h q) -> p h q", q=HD), sf.rearrange("p (h q) -> p h q", q=HD),
                                                       etot[:, hh * 16:(hh + 1) * 16].unsqueeze(2).to_broadcast([P, 16, HD]), ALU.mult),
                     reads=[self.S_f.k, sm.k], writes=[self.S_f.k])
                for q in range(2):
                    sq_ = self.S_f.t[:, hh * 1024 + q * 512:hh * 1024 + (q + 1) * 512]
                    S.op("dve", lambda e: e.tensor_tensor(sq_, sq_, pst[q].t[:], ALU.add), reads=[pst[q].k, self.S_f.k], writes=[self.S_f.k])
                if not lite and "fs" not in self.skip:
                    S.op("dve", lambda e: e.tensor_copy(self.S_b.t[:, cs_], sf), reads=[self.S_f.k], writes=[self.S_b.k])
        if not lite and "f3" not in self.skip:
            wo = [self.sb(f"wo{i}", [P, 16, P], BF16) for i in range(2)]
            tks += [b.k for b in wo]
            for dq in range(KD):
                b = self.wctr % 2
                self.wctr += 1
                w = wo[b]
                S.dma("sp", self.q_wo[b], [(w.t[:], self.w_out_b[dq].rearrange("p (j d) -> p j d", j=16))],
                      reads=[self.woutb_k[dq]], writes=[w.k])
                pb = B[dq % 2]
                for j in range(16):
                    S.op("pe", lambda e: e.matmul(pb.t[:, :ntok], lhsT=w.t[:, j, :], rhs=ynT.t[:, j, :], start=(j == 0), stop=(j == 15)),
                         reads=[w.k, ynT.k], writes=[pb.k], inc=(j == 15))
                xs_ = self.xT.t[:, dq, c0:c0 + ntok]
                S.op("dve", lambda e: e.tensor_tensor(xs_, xs_, pb.t[:, :ntok], ALU.add), reads=[pb.k] + self.xtk([dq], c0, ntok), writes=self.xtk([dq], c0, ntok))
        self.release_after(m, tks)


def _const_tables():
    i = np.arange(P)
    ident = np.eye(P, dtype=np.float32)
    tri = (i[:, None] <= i[None, :]).astype(np.float32)
    lmat = (i[:, None] > i[None, :]).astype(np.float32)
    ones = np.ones((P, P), np.float32)
    onesmean = np.full((P, P), 1.0 / D, np.float32)
    return np.stack([ident, tri, lmat, ones, onesmean])


def _bands(special):
    s = np.arange(P)[:, None]
    t = np.arange(P)[None, :]
    cur = np.zeros((4, P, P), np.float32)
    prev = np.zeros((4, P, P), np.float32)
    for g, w in enumerate(POOL_WINDOWS):
        cnt = np.minimum(t + 1, w).astype(np.float32) if special else np.full((1, P), float(w), np.float32)
        inwin = ((s <= t) & (s > t - w)).astype(np.float32)
        cur[g] = inwin / cnt - (s == t).astype(np.float32)
        prev[g] = ((s - P) > (t - w)).astype(np.float32) / cnt
    return cur, prev


_CACHE = {}


def _get_nc(stop=None):
    if stop not in _CACHE:
        _CACHE[stop] = Builder(stop).build()
    return _CACHE[stop]


def kernel(x, pool_norm_w, pool_w, pool_scale, dense_norm_w, dense_w_gate, dense_w_up, dense_w_down,
           ssd_norm_w, ssd_w_in, ssd_conv_w, ssd_conv_b, ssd_dt_bias, ssd_a_log, ssd_d, ssd_gate_norm_w,
           ssd_w_out, moe_norm_w, moe_w_router, moe_w_gate, moe_w_up, moe_w_down, final_norm_w, _stop=None):
    f = lambda a: np.ascontiguousarray(np.asarray(a, dtype=np.float32))
    x = f(x)
    n_cores = 8
    def colmajor(v, nk):
        return f(v).reshape(nk, P).T
    cols = np.zeros((P, NCOLS), np.float32)
    cols[:, C_DNW:C_DNW + 8] = colmajor(dense_norm_w[0], 8)
    cols[:, C_SNW:C_SNW + 8] = colmajor(ssd_norm_w[0], 8)
    cols[:, C_MNW:C_MNW + 8] = colmajor(moe_norm_w[0], 8)
    cw = f(ssd_conv_w[0])
    cols[:, C_CW:C_CW + 128] = cw.reshape(4, 32, P).transpose(2, 1, 0).reshape(P, 128)
    cols[:, C_CB:C_CB + 32] = colmajor(ssd_conv_b[0], 32)
    cols[:, C_GNW:C_GNW + 16] = colmajor(ssd_gate_norm_w[0], 16)
    reps = np.zeros((P, NREPS), np.float32)
    big = np.zeros((P, 3 * D), np.float32)
    big[:, R_PNW:R_PNW + D] = f(pool_norm_w[0])[None, :]
    big[:, R_FNW:R_FNW + D] = f(final_norm_w)[None, :]
    big[:, R_PSC:R_PSC + D] = f(pool_scale[0])[None, :]
    reps[:, R_DTB:R_DTB + NH] = f(ssd_dt_bias[0])[None, :]
    reps[:, R_ALOG:R_ALOG + NH] = f(ssd_a_log[0])[None, :]
    reps[:, R_D:R_D + NH] = f(ssd_d[0])[None, :]
    csq = _const_tables()
    gcur, gprev = _bands(False)
    scur, _ = _bands(True)
    shared = {
        "c_sq": csq, "c_reps": reps, "c_big": big, "pool_w": f(pool_w[0]),
        "dense_w_gate": f(dense_w_gate[0]), "dense_w_up": f(dense_w_up[0]), "dense_w_down": f(dense_w_down[0]),
        "ssd_w_in": f(ssd_w_in[0]), "ssd_w_out": f(ssd_w_out[0]), "moe_w_router": f(moe_w_router[0]),
        "moe_w_gate": f(moe_w_gate[0]), "moe_w_up": f(moe_w_up[0]), "moe_w_down": f(moe_w_down[0]),
    }
    if _stop is not None and _stop.split(",")[0] not in ("", "nopre", "two", "two1"):
        for k_ in ("moe_w_gate", "moe_w_up", "moe_w_down"):
            del shared[k_]
    in_maps = []
    for c in range(n_cores):
        b, h = c // 2, c % 2
        own = x[b, h * NTOK:(h + 1) * NTOK]
        pre = x[b, 0:NPRE] if h == 1 else np.zeros((NPRE, D), np.float32)
        cc = cols.copy()
        cc[:, C_FLAG] = float(h)
        band = np.stack([gcur, gprev, scur if h == 1 else gcur, scur if h == 0 else gcur])
        m = dict(shared)
        m["xs"] = np.ascontiguousarray(np.concatenate([pre, own], axis=0))
        m["c_cols"] = cc
        m["c_band"] = np.ascontiguousarray(band)
        in_maps.append(m)
    if _stop in ("two", "two1"):
        cores = list(range(n_cores)) if _stop == "two" else [1]
        ncA = _get_nc("ssd")
        ncB = _get_nc("moe2")
        keysA = ("c_sq", "c_reps", "c_big", "pool_w", "dense_w_gate", "dense_w_up", "dense_w_down", "ssd_w_in",
                 "ssd_w_out", "moe_w_router", "xs", "c_cols", "c_band")
        keysB = ("c_sq", "c_reps", "c_big", "moe_w_router", "moe_w_gate", "moe_w_up", "moe_w_down", "c_cols")
        resA = run_bass_kernel_spmd(ncA, [{k_: in_maps[c][k_] for k_ in keysA} for c in cores], core_ids=list(range(len(cores))))
        mapsB = []
        for i, c in enumerate(cores):
            mb = {k_: in_maps[c][k_] for k_ in keysB}
            mb["xs"] = np.ascontiguousarray(resA.results[i]["out"])
            mapsB.append(mb)
        res = run_bass_kernel_spmd(ncB, mapsB, core_ids=list(range(len(cores))))
        if _stop == "two1":
            return res.results[0]["out"]
        out = np.empty((4, 8192, D), np.float32)
        for c in range(n_cores):
            b, h = c // 2, c % 2
            out[b, h * NTOK:(h + 1) * NTOK] = res.results[c]["out"]
        return out
    nc = _get_nc(_stop)
    if _stop is not None and "one" in _stop:
        res1 = run_bass_kernel_spmd(nc, in_maps[1:2], core_ids=[0], trace=("trace" in _stop))
        if "trace" in _stop:
            print("EXEC_TIME_NS", res1.exec_time_ns, flush=True)
        return res1.results[0]["out"]
    res = run_bass_kernel_spmd(nc, in_maps, core_ids=list(range(n_cores)))
    out = np.empty((4, 8192, D), np.float32)
    for c in range(n_cores):
        b, h = c // 2, c % 2
        out[b, h * NTOK:(h + 1) * NTOK] = res.results[c]["out"]
    return out
```

```python
import numpy as np
import concourse.bass as bass
import concourse.mybir as mybir
from concourse.bass_utils import run_bass_kernel_spmd

F32 = mybir.dt.float32
BF16 = mybir.dt.bfloat16
AF = mybir.ActivationFunctionType
ALU = mybir.AluOpType

P = 128
D = 1024
KD = 8
DFF = 2816
DFE = 3584
NE = 8
DIN = 2048
NH = 32
HD = 64
NG = 8
NTOK = 4096
NPRE = 4096
HALF = 2048
POOL_WINDOWS = (2, 4, 8, 16)
EPS = 1e-6
SSM_EPS = 1e-5

C_DNW, C_SNW, C_MNW, C_CW, C_CB, C_GNW, C_FLAG = 0, 8, 16, 24, 152, 184, 200
NCOLS = 201
R_PNW, R_FNW, R_PSC = 0, 1024, 2048
R_DTB, R_ALOG, R_D = 0, 32, 64
NREPS = 96


class Tk:
    __slots__ = ("name", "w", "r")

    def __init__(self, name=""):
        self.name = name
        self.w = None
        self.r = {}


class Slot:
    def __init__(self, S, name):
        self.key = "dma_" + name
        self.sem = S.nc.alloc_semaphore(name="q_" + name)
        self.cnt = 0


class Sched:
    def __init__(self, nc):
        self.nc = nc
        self.engs = {}
        for name, e in [("pe", nc.tensor), ("act", nc.scalar), ("dve", nc.vector),
                        ("pool", nc.gpsimd), ("sp", nc.sync)]:
            self.engs[name] = dict(e=e, key=name, sem=nc.alloc_semaphore(name="s_" + name),
                                   cnt=0, waited={})
        self.n_ops = 0
        self.n_wait = 0

    def slot(self, name):
        return Slot(self, name)

    def _wait_deps(self, E, reads, writes):
        deps = {}

        def add(d):
            if d is None:
                return
            k = d[0]
            if k not in deps or deps[k][2] < d[2]:
                deps[k] = d
        for t in reads:
            add(t.w)
        for t in writes:
            add(t.w)
            for d in t.r.values():
                add(d)
        for k, (key, sem, val) in deps.items():
            if key == E["key"] and key == "pe":
                continue
            if E["waited"].get(key, 0) >= val:
                continue
            if key == E["key"]:
                assert val <= E["cnt"], "own-engine dep on pending instruction"
            E["e"].wait_ge(sem, val)
            E["waited"][key] = val
            self.n_wait += 1

    def _mark(self, my, reads, writes):
        for t in reads:
            old = t.r.get(my[0])
            if old is None or old[2] < my[2]:
                t.r[my[0]] = my
        for t in writes:
            t.w = my
            t.r = {}

    def op(self, eng, fn, reads=(), writes=(), inc=True):
        E = self.engs[eng]
        self._wait_deps(E, reads, writes)
        ins = fn(E["e"])
        self.n_ops += 1
        if inc:
            E["cnt"] += 1
            ins.then_inc(E["sem"], 1)
            my = (E["key"], E["sem"], E["cnt"])
        else:
            my = (E["key"], E["sem"], E["cnt"] + 1)
        self._mark(my, reads, writes)
        return ins

    def dma(self, eng, slot, pairs, reads=(), writes=()):
        E = self.engs[eng]
        self._wait_deps(E, reads, writes)
        for (o, i) in pairs:
            ins = E["e"].dma_start(out=o, in_=i)
            slot.cnt += 16
            ins.then_inc(slot.sem, 16)
            self.n_ops += 1
        my = (slot.key, slot.sem, slot.cnt)
        self._mark(my, reads, writes)

    def wait_all(self, eng, tks):
        self._wait_deps(self.engs[eng], [], tks)


class Buf:
    def __init__(self, t, name):
        self.t = t
        self.k = Tk(name)


class Ring:
    def __init__(self, bufs):
        self.bufs = bufs
        self.i = 0

    def next(self):
        b = self.bufs[self.i % len(self.bufs)]
        self.i += 1
        return b


class Builder:
    def __init__(self, stop=None):
        flags = (stop or "").split(",")
        self.small = "small" in flags
        self.skip = set(flags)
        self.nolite = "nolite" in flags
        self.nofull = "nofull" in flags
        stop = flags[0] if flags[0] else None
        self.stop = stop
        nc = self.nc = bass.Bass("TRN2", target_bir_lowering=False)
        self.S = Sched(nc)
        self.ctx = []
        dt = nc.dram_tensor

        def inp(name, shape):
            return dt(name, list(shape), F32, kind="ExternalInput").ap()
        self.c_sq = inp("c_sq", [5, P, P])
        self.c_cols = inp("c_cols", [P, NCOLS])
        self.c_reps = inp("c_reps", [P, NREPS])
        self.c_big = inp("c_big", [P, 3 * D])
        self.w_r = inp("moe_w_router", [D, NE])
        if stop == "moe2":
            self.xs = inp("xs", [NTOK, D])
        else:
            self.xs = inp("xs", [NPRE + NTOK, D])
            self.c_band = inp("c_band", [4, 4, P, P])
            self.pool_w = inp("pool_w", [4, 256, 256])
            self.w_dg = inp("dense_w_gate", [D, DFF])
            self.w_du = inp("dense_w_up", [D, DFF])
            self.w_dd = inp("dense_w_down", [DFF, D])
            self.w_in = inp("ssd_w_in", [D, 6176])
            self.w_out = inp("ssd_w_out", [DIN, D])
            self.w_in_b = dt("w_in_b", [24, P, KD * 256], BF16).ap()
            self.w_out_b = dt("w_out_b", [KD, P, 16 * P], BF16).ap()
            self.winb_k = [Tk(f"winb{t}") for t in range(24)]
            self.woutb_k = [Tk(f"woutb{t}") for t in range(KD)]
        if stop is None or stop in ("nopre", "moe2"):
            self.w_mg = inp("moe_w_gate", [NE, D, DFE])
            self.w_mu = inp("moe_w_up", [NE, D, DFE])
            self.w_md = inp("moe_w_down", [NE, DFE, D])
        self.out = dt("out", [NTOK, D], F32, kind="ExternalOutput").ap()

    def slot(self, name):
        if name not in self._slots:
            self._slots[name] = self.S.slot(name)
        return self._slots[name]

    def xtk(self, ks, c0, n):
        return [self.xk[k][b] for k in ks for b in range(c0 // P, (c0 + n + P - 1) // P)]

    def sb(self, name, shape, dtype):
        self.uid = getattr(self, "uid", 0) + 1
        name = f"{name}_{self.uid}"
        g = self.nc.sbuf_tensor(name, list(shape), dtype)
        t = g.__enter__()
        self.ctx.append(g)
        return Buf(t, name)

    def ps(self, name, shape, dtype=F32):
        g = self.nc.psum_tensor(name, list(shape), dtype)
        t = g.__enter__()
        self.ctx.append(g)
        return Buf(t, name)

    def mark(self):
        return len(self.ctx)

    def release(self, m):
        while len(self.ctx) > m:
            self.ctx.pop().__exit__(None, None, None)

    def build(self):
        S = self.S
        self._slots = {}
        self.q_w = [self.slot("w0"), self.slot("w1")]
        self.q_w2 = [self.slot("v0"), self.slot("v1")]
        self.q_wo = [self.slot("o0"), self.slot("o1")]
        self.wctr = 0
        self.xk = [[Tk(f"xT{k}_{b}") for b in range(HALF // P)] for k in range(KD)]
        if self.stop == "moe2":
            return self.build_moe2()
        self.setup()
        self.PS = self.ps("psum", [P, 8, 512])
        self.B = [Buf(self.PS.t[:, i, :], f"bank{i}") for i in range(8)]
        self.xT = self.sb("xT", [P, KD, HALF], F32)
        self.htok = [self.sb(f"htok{i}", [P, D], BF16) for i in range(3)]
        self.hidx = 0
        self.halo = self.sb("halo", [P, 32, 3], F32)
        self.S_f = self.sb("S_f", [P, DIN], F32)
        self.S_b = self.sb("S_b", [P, DIN], BF16)
        S.op("pool", lambda e: e.memset(self.halo.t[:], 0.0), writes=[self.halo.k])
        S.op("pool", lambda e: e.memset(self.S_f.t[:], 0.0), writes=[self.S_f.k])
        S.op("pool", lambda e: e.memset(self.htok[2].t[:], 0.0), writes=[self.htok[2].k])
        self.hprev = self.htok[2]

        if self.stop != "nopre":
            ngrp = NPRE // 1024
            for grp in range(1 if self.small else ngrp):
                r0 = grp * 1024
                self.pool_phase(r0, 1024, first=("pre0" if grp == 0 else None))
                if self.stop == "pool":
                    continue
                self.ffn_dense(1024)
                if self.stop == "ffn":
                    continue
                for sg in range(2):
                    if self.nolite:
                        continue
                    self.ssd_group(sg * 512, 512, lite=True, with_c=(grp == ngrp - 1 and sg == 1))
        S.op("dve", lambda e: e.tensor_scalar(self.S_f.t[:], self.S_f.t[:], self.cols.t[:, C_FLAG:C_FLAG + 1], None,
                                              op0=ALU.mult), reads=[self.S_f.k, self.cols.k], writes=[self.S_f.k])
        S.op("act", lambda e: e.copy(self.S_b.t[:], self.S_f.t[:]), reads=[self.S_f.k], writes=[self.S_b.k])

        for half in range(1 if self.small else 2):
            r0 = NPRE + half * HALF
            self.pool_phase(r0, HALF, first=("main0" if half == 0 else None))
            if self.stop == "pool":
                self.final_phase(half, norm=False)
                continue
            for tg in range(1 if self.small else HALF // 1024):
                self.ffn_dense(1024, c0=tg * 1024)
            if self.stop == "ffn":
                self.final_phase(half, norm=False)
                continue
            for sg in range(1 if self.small else HALF // 256):
                if self.nofull:
                    continue
                self.ssd_group(sg * 256, 256, lite=False)
            if self.stop == "ssd":
                self.final_phase(half, norm=False)
                continue
            self.moe_half()
            self.final_phase(half, norm=("mn" not in self.skip))
        if not self.small:
            S.wait_all("sp", [self.otk[0], self.otk[1]])
        self.release(0)
        return self.nc

    def build_moe2(self):
        S = self.S
        self.csq = self.sb("csq", [P, 5, P], F32)
        self.ident_f = self.csq.t[:, 0, :]
        self.tri_f = self.csq.t[:, 1, :]
        self.lmat_f = self.csq.t[:, 2, :]
        self.ones_f = self.csq.t[:, 3, :]
        self.onesmean_f = self.csq.t[:, 4, :]
        self.cols = self.sb("cols", [P, NCOLS], F32)
        self.wr = self.sb("wr", [P, KD, NE], F32)
        S.dma("sp", self.slot("c1"), [(self.csq.t[:], self.c_sq.rearrange("c p q -> p c q")),
                                      (self.cols.t[:], self.c_cols),
                                      (self.wr.t[:], self.w_r.rearrange("(k p) e -> p k e", p=P))],
              writes=[self.csq.k, self.cols.k, self.wr.k])
        self.PS = self.ps("psum", [P, 8, 512])
        self.B = [Buf(self.PS.t[:, i, :], f"bank{i}") for i in range(8)]
        self.xT = self.sb("xT", [P, KD, HALF], F32)
        for half in range(2):
            self.load_phase(half)
            self.moe_half()
            self.final_phase(half, norm=True)
        S.wait_all("sp", [self.otk[0], self.otk[1]])
        self.release(0)
        return self.nc

    def load_phase(self, half):
        S, B = self.S, self.B
        m = self.mark()
        xtok = Ring([self.sb(f"xtok{i}", [P, D], F32) for i in range(2)])
        for ti in range(HALF // P):
            xt = xtok.next()
            c0 = ti * P
            r = half * HALF + c0
            S.dma("sp", self.slot("x%d" % (ti % 2)), [(xt.t[:], self.xs[r:r + P, :])], writes=[xt.k])
            for k in range(KD):
                bk = B[k // 4]
                S.op("pe", lambda e: e.transpose(bk.t[:, (k % 4) * P:(k % 4 + 1) * P], xt.t[:, k * P:(k + 1) * P], self.ident_f),
                     reads=[xt.k, self.csq.k], writes=[bk.k], inc=(k % 4 == 3))
            for hb in range(2):
                S.op("act", lambda e: e.copy(self.xT.t[:, hb * 4:(hb + 1) * 4, c0:c0 + P],
                                             B[hb].t[:].rearrange("p (k t) -> p k t", k=4)),
                     reads=[B[hb].k], writes=self.xtk(range(hb * 4, hb * 4 + 4), c0, P))
        self.release_after(m, [xtok.bufs[0].k, xtok.bufs[1].k])

    def setup(self):
        S, nc = self.S, self.nc
        self.csq = self.sb("csq", [P, 5, P], F32)
        self.ident_f = self.csq.t[:, 0, :]
        self.tri_f = self.csq.t[:, 1, :]
        self.lmat_f = self.csq.t[:, 2, :]
        self.ones_f = self.csq.t[:, 3, :]
        self.onesmean_f = self.csq.t[:, 4, :]
        self.cols = self.sb("cols", [P, NCOLS], F32)
        self.reps = self.sb("reps", [P, NREPS], F32)
        self.identb = self.sb("identb", [P, P], BF16)
        self.bands = self.sb("bands", [P, 4, 4, P], BF16)
        self.poolw = self.sb("poolw", [P, 4, 2, 256], BF16)
        self.wdt = self.sb("wdt", [P, KD, NH], BF16)
        self.wr = self.sb("wr", [P, KD, NE], F32)
        self.aneg = self.sb("aneg", [P, NH], F32)
        S.dma("sp", self.slot("c1"), [(self.csq.t[:], self.c_sq.rearrange("c p q -> p c q")),
                               (self.cols.t[:], self.c_cols), (self.reps.t[:], self.c_reps),
                               (self.wr.t[:], self.w_r.rearrange("(k p) e -> p k e", p=P))],
              writes=[self.csq.k, self.cols.k, self.reps.k, self.wr.k])
        S.dma("pool", self.slot("c2"), [(self.bands.t[:], self.c_band.rearrange("a g p q -> p a g q")),
                                 (self.wdt.t[:], self.w_in[:, 6144:6176].rearrange("(k p) n -> p k n", p=P))],
              writes=[self.bands.k, self.wdt.k])
        S.op("dve", lambda e: e.tensor_copy(self.identb.t[:], self.ident_f), reads=[self.csq.k], writes=[self.identb.k])
        S.op("act", lambda e: e.activation(self.aneg.t[:], self.reps.t[:, R_ALOG:R_ALOG + NH], AF.Exp),
             reads=[self.reps.k], writes=[self.aneg.k])
        S.op("dve", lambda e: e.tensor_scalar(self.aneg.t[:], self.aneg.t[:], -1.0, None, op0=ALU.mult),
             reads=[self.aneg.k], writes=[self.aneg.k])
        m = self.mark()
        pw32 = self.sb("pw32", [P, 4, 2, 256], F32)
        pscb = self.sb("pscb", [P, D], F32)
        S.dma("sp", self.slot("c3"), [(pw32.t[:], self.pool_w.rearrange("g (kc p) e -> p g kc e", p=P)),
                                      (pscb.t[:], self.c_big[:, R_PSC:R_PSC + D])], writes=[pw32.k, pscb.k])
        psc = pscb.t[:].rearrange("p (g e) -> p g e", g=4)
        for kc in range(2):
            S.op("dve", lambda e: e.tensor_tensor(self.poolw.t[:, :, kc, :], pw32.t[:, :, kc, :], psc, ALU.mult),
                 reads=[pw32.k, pscb.k], writes=[self.poolw.k])
        self.release_after(m, [pw32.k, pscb.k])
        m = self.mark()
        stg = Ring([self.sb(f"stg{i}", [P, KD * 256], BF16) for i in range(4)])
        for t in range(24 + KD):
            st_ = stg.next()
            if t < 24:
                src = self.w_in[:, t * 256:(t + 1) * 256].rearrange("(k p) n -> p k n", p=P)
                dst, dk_ = self.w_in_b[t], self.winb_k[t]
                sview = st_.t[:].rearrange("p (k n) -> p k n", k=KD)
            else:
                dq = t - 24
                src = self.w_out[:, dq * P:(dq + 1) * P].rearrange("(j p) d -> p j d", p=P)
                dst, dk_ = self.w_out_b[dq], self.woutb_k[dq]
                sview = st_.t[:].rearrange("p (j d) -> p j d", j=16)
            S.dma("pool", self.slot("pc%d" % (t % 4)), [(sview, src)], writes=[st_.k])
            S.dma("sp", self.slot("pd%d" % (t % 4)), [(dst, st_.t[:])], reads=[st_.k], writes=[dk_])
        self.release_after(m, [b_.k for b_ in stg.bufs])

    def pool_phase(self, r0, ntok, first):
        S = self.S
        m = self.mark()
        xtok = Ring([self.sb(f"xtok{i}", [P, D], F32) for i in range(2)])
        junk = self.sb("pjunk", [P, D], BF16)
        pT = self.sb("pT", [P, KD, P], BF16)
        st = Ring([self.sb(f"pst{i}", [P, 4], F32) for i in range(2)])
        pnwb = self.sb("pnwb", [P, D], F32)
        S.dma("sp", self.slot("c4"), [(pnwb.t[:], self.c_big[:, R_PNW:R_PNW + D])], writes=[pnwb.k])
        pnw = pnwb.t[:]
        B = self.B
        for ti in range(ntok // P):
            xt = xtok.next()
            s4 = st.next()
            c0 = ti * P
            S.dma("sp", self.slot("x%d" % (ti % 2)), [(xt.t[:], self.xs[r0 + c0:r0 + c0 + P, :])], writes=[xt.k])
            S.op("act", lambda e: e.activation(junk.t[:], xt.t[:], AF.Square, accum_out=s4.t[:, 0:1]),
                 reads=[xt.k], writes=[junk.k, s4.k])
            S.op("act", lambda e: e.activation(s4.t[:, 1:2], s4.t[:, 0:1], AF.Sqrt, bias=EPS, scale=1.0 / D),
                 reads=[s4.k], writes=[s4.k])
            S.op("dve", lambda e: e.reciprocal(s4.t[:, 2:3], s4.t[:, 1:2]), reads=[s4.k], writes=[s4.k])
            hc = self.htok[self.hidx % 2]
            self.hidx += 1
            S.op("dve", lambda e: e.scalar_tensor_tensor(hc.t[:], xt.t[:], s4.t[:, 2:3], pnw, op0=ALU.mult, op1=ALU.mult),
                 reads=[xt.k, s4.k, pnwb.k], writes=[hc.k])
            for k in range(KD):
                bk = B[k // 4]
                S.op("pe", lambda e: e.transpose(bk.t[:, (k % 4) * P:(k % 4 + 1) * P], xt.t[:, k * P:(k + 1) * P], self.ident_f),
                     reads=[xt.k, self.csq.k], writes=[bk.k], inc=(k % 4 == 3))
            for hb in range(2):
                S.op("act", lambda e: e.copy(self.xT.t[:, hb * 4:(hb + 1) * 4, c0:c0 + P],
                                             B[hb].t[:].rearrange("p (k t) -> p k t", k=4)),
                     reads=[B[hb].k], writes=self.xtk(range(hb * 4, hb * 4 + 4), c0, P))
            kind_cur = 0
            if ti == 0 and first == "pre0":
                kind_cur = 2
            if ti == 0 and first == "main0":
                kind_cur = 3
            hp = self.hprev
            for k in range(KD):
                g = k // 2
                bk = B[2 + k // 4]
                o = bk.t[:, (k % 4) * P:(k % 4 + 1) * P]
                S.op("pe", lambda e: e.matmul(o, lhsT=hp.t[:, k * P:(k + 1) * P], rhs=self.bands.t[:, 1, g, :], start=True, stop=False),
                     reads=[hp.k, self.bands.k], writes=[bk.k], inc=False)
                S.op("pe", lambda e: e.matmul(o, lhsT=hc.t[:, k * P:(k + 1) * P], rhs=self.bands.t[:, kind_cur, g, :], start=False, stop=True),
                     reads=[hc.k, self.bands.k], writes=[bk.k], inc=(k % 4 == 3))
            for hb in range(2):
                S.op("act", lambda e: e.copy(pT.t[:, hb * 4:(hb + 1) * 4, :], B[2 + hb].t[:].rearrange("p (k t) -> p k t", k=4)),
                     reads=[B[2 + hb].k], writes=[pT.k])
            for k in range(KD):
                g, ec = k // 2, k % 2
                bk = B[4 + k // 4]
                o = bk.t[:, (k % 4) * P:(k % 4 + 1) * P]
                for kc in range(2):
                    S.op("pe", lambda e: e.matmul(o, lhsT=self.poolw.t[:, g, kc, ec * P:(ec + 1) * P], rhs=pT.t[:, 2 * g + kc, :],
                                                  start=(kc == 0), stop=(kc == 1)),
                         reads=[self.poolw.k, pT.k], writes=[bk.k], inc=(k % 4 == 3 and kc == 1))
            for hb in range(2):
                xs_ = self.xT.t[:, hb * 4:(hb + 1) * 4, c0:c0 + P]
                S.op("dve", lambda e: e.tensor_tensor(xs_, B[4 + hb].t[:].rearrange("p (k t) -> p k t", k=4), xs_, ALU.add),
                     reads=[B[4 + hb].k] + self.xtk(range(hb * 4, hb * 4 + 4), c0, P), writes=self.xtk(range(hb * 4, hb * 4 + 4), c0, P))
            self.hprev = hc
        self.release_after(m, [xtok.bufs[0].k, xtok.bufs[1].k, junk.k, pT.k, st.bufs[0].k, st.bufs[1].k, pnwb.k])

    def release_after(self, m, tks):
        for eng in ("pe", "act", "dve", "pool", "sp"):
            self.S.wait_all(eng, tks)
        self.release(m)

    def norm_fm(self, c0, n, nwcol, hn, hn_c0, scratch, eps=EPS):
        S = self.S
        sq, sd, rs = scratch
        bst = self.B[6]
        for k in range(KD):
            q = sq.next()
            S.op("act", lambda e: e.activation(q.t[:, :n], self.xT.t[:, k, c0:c0 + n], AF.Square),
                 reads=self.xtk([k], c0, n), writes=[q.k])
            S.op("pe", lambda e: e.matmul(bst.t[:, :n], lhsT=self.onesmean_f, rhs=q.t[:, :n], start=(k == 0), stop=(k == KD - 1)),
                 reads=[q.k, self.csq.k], writes=[bst.k])
        S.op("act", lambda e: e.activation(sd.t[:, :n], bst.t[:, :n], AF.Sqrt, bias=eps, scale=1.0),
             reads=[bst.k], writes=[sd.k])
        S.op("dve", lambda e: e.reciprocal(rs.t[:, :n], sd.t[:, :n]), reads=[sd.k], writes=[rs.k])
        for k in range(KD):
            S.op("dve", lambda e: e.scalar_tensor_tensor(hn.t[:, k, hn_c0:hn_c0 + n], self.xT.t[:, k, c0:c0 + n],
                                                         self.cols.t[:, nwcol + k:nwcol + k + 1], rs.t[:, :n],
                                                         op0=ALU.mult, op1=ALU.mult),
                 reads=self.xtk([k], c0, n) + [self.cols.k, rs.k], writes=[hn.k])

    def norm_scratch(self, w=512):
        sq = Ring([self.sb(f"nsq{i}", [P, w], F32) for i in range(2)])
        sd = self.sb("nsd", [P, w], F32)
        rs = self.sb("nrs", [P, w], F32)
        return (sq, sd, rs), [sq.bufs[0].k, sq.bufs[1].k, sd.k, rs.k]

    def ffn_bufs(self):
        wg = [self.sb(f"wg{i}", [P, KD, 512], BF16) for i in range(2)]
        wu = [self.sb(f"wu{i}", [P, KD, 512], BF16) for i in range(2)]
        wd = [self.sb(f"wd{i}", [P, 4, D], BF16) for i in range(2)]
        sg = Ring([self.sb(f"sg{i}", [P, 2, 512], F32) for i in range(2)])
        act = Ring([self.sb(f"act{i}", [P, 4, 512], BF16) for i in range(2)])
        tks = [b.k for b in wg + wu + wd + sg.bufs + act.bufs]
        return dict(wg=wg, wu=wu, wd=wd, sg=sg, act=act), tks

    def ffn_pass(self, fb, hn, ntok, c0, g_ap, u_ap, d_ap, F, comb=None, flush=True):
        S, B, PS = self.S, self.B, self.PS
        f0 = 0
        pending = None
        while f0 < F:
            W = min(512, F - f0)
            ncx = W // P
            b = self.wctr % 2
            self.wctr += 1
            wg, wu, wd = fb["wg"][b], fb["wu"][b], fb["wd"][b]
            S.dma("pool", self.q_w[b],
                  [(wg.t[:, :, :W], g_ap[:, f0:f0 + W].rearrange("(k p) n -> p k n", p=P)),
                   (wu.t[:, :, :W], u_ap[:, f0:f0 + W].rearrange("(k p) n -> p k n", p=P)),
                   (wd.t[:, :ncx, :], d_ap[f0:f0 + W, :].rearrange("(c p) d -> p c d", p=P))],
                  writes=[wg.k, wu.k, wd.k])
            for s0 in range(0, ntok, 512):
                n = min(512, ntok - s0)
                a = fb["act"].next()
                for cp in range(ncx // 2):
                    for (w, bb) in ((wg, 0), (wu, 2)):
                        for c2 in range(2):
                            c = 2 * cp + c2
                            pb = B[bb + c2]
                            for k in range(KD):
                                S.op("pe", lambda e: e.matmul(pb.t[:, :n], lhsT=w.t[:, k, c * P:(c + 1) * P], rhs=hn.t[:, k, s0:s0 + n],
                                                              start=(k == 0), stop=(k == KD - 1)),
                                     reads=[w.k, hn.k], writes=[pb.k], inc=(k == KD - 1))
                    sg = fb["sg"].next()
                    S.op("act", lambda e: e.activation(sg.t[:, :, :n], PS.t[:, 0:2, :n], AF.Silu),
                         reads=[B[0].k, B[1].k], writes=[sg.k])
                    if comb is None:
                        S.op("dve", lambda e: e.tensor_tensor(a.t[:, 2 * cp:2 * cp + 2, :n], sg.t[:, :, :n], PS.t[:, 2:4, :n], ALU.mult),
                             reads=[sg.k, B[2].k, B[3].k], writes=[a.k])
                    else:
                        S.op("dve", lambda e: e.tensor_tensor(sg.t[:, :, :n], sg.t[:, :, :n], PS.t[:, 2:4, :n], ALU.mult),
                             reads=[sg.k, B[2].k, B[3].k], writes=[sg.k])
                        S.op("dve", lambda e: e.tensor_tensor(a.t[:, 2 * cp:2 * cp + 2, :n], sg.t[:, :, :n],
                                                              comb.t[:, s0:s0 + n].unsqueeze(1).to_broadcast([P, 2, n]), ALU.mult),
                             reads=[sg.k, comb.k], writes=[a.k])
                if pending is not None:
                    pending()
                pending = self._down_closure(a, wd, ncx, n, c0 + s0)
            f0 += W
        if not flush:
            return pending
        if pending is not None:
            pending()

    def _down_closure(self, a, wd, ncx, n, col0):
        S, B, PS = self.S, self.B, self.PS

        def run():
            for dp in range(4):
                ob = 4 + 2 * (dp % 2)
                for d2 in range(2):
                    dk = 2 * dp + d2
                    po = B[ob + d2]
                    for c in range(ncx):
                        S.op("pe", lambda e: e.matmul(po.t[:, :n], lhsT=wd.t[:, c, dk * P:(dk + 1) * P], rhs=a.t[:, c, :n],
                                                      start=(c == 0), stop=(c == ncx - 1)),
                             reads=[wd.k, a.k], writes=[po.k], inc=(c == ncx - 1))
                xs_ = self.xT.t[:, 2 * dp:2 * dp + 2, col0:col0 + n]
                xk_ = self.xtk([2 * dp, 2 * dp + 1], col0, n)
                S.op("dve", lambda e: e.tensor_tensor(xs_, PS.t[:, ob:ob + 2, :n], xs_, ALU.add),
                     reads=[B[ob].k, B[ob + 1].k] + xk_, writes=xk_)
        return run

    def ffn_dense(self, ntok, c0=0):
        m = self.mark()
        hn = self.sb("hn_d", [P, KD, ntok], BF16)
        scr, stk = self.norm_scratch()
        fb, ftk = self.ffn_bufs()
        for s0 in range(0, ntok, 512):
            self.norm_fm(c0 + s0, 512, C_DNW, hn, s0, scr)
        self.ffn_pass(fb, hn, ntok, c0, self.w_dg, self.w_du, self.w_dd, DFF)
        self.release_after(m, [hn.k] + stk + ftk)

    def moe_half(self):
        S, B = self.S, self.B
        m = self.mark()
        hn = self.sb("hn_m", [P, KD, HALF], BF16)
        comb_tok = self.sb("comb_tok", [P, HALF // P, NE], F32)
        comb = self.sb("comb_rep", [P, HALF], F32)
        m2 = self.mark()
        scr, stk = self.norm_scratch(512)
        sq, sd, rs = scr
        hf = self.sb("hf", [P, KD, 512], F32)
        sm = Ring([self.sb(f"rsm{i}", [P, 48], F32) for i in range(2)])
        bst, blg = B[6], B[7]
        for s0 in range(0, HALF, 512):
            n = 512
            for k in range(KD):
                q = sq.next()
                S.op("act", lambda e: e.activation(q.t[:, :n], self.xT.t[:, k, s0:s0 + n], AF.Square),
                     reads=self.xtk([k], s0, n), writes=[q.k])
                S.op("pe", lambda e: e.matmul(bst.t[:, :n], lhsT=self.onesmean_f, rhs=q.t[:, :n], start=(k == 0), stop=(k == KD - 1)),
                     reads=[q.k, self.csq.k], writes=[bst.k])
            S.op("act", lambda e: e.activation(sd.t[:, :n], bst.t[:, :n], AF.Sqrt, bias=EPS, scale=1.0), reads=[bst.k], writes=[sd.k])
            S.op("dve", lambda e: e.reciprocal(rs.t[:, :n], sd.t[:, :n]), reads=[sd.k], writes=[rs.k])
            for k in range(KD):
                S.op("dve", lambda e: e.scalar_tensor_tensor(hf.t[:, k, :], self.xT.t[:, k, s0:s0 + n],
                                                             self.cols.t[:, C_MNW + k:C_MNW + k + 1], rs.t[:, :n],
                                                             op0=ALU.mult, op1=ALU.mult),
                     reads=self.xtk([k], s0, n) + [self.cols.k, rs.k], writes=[hf.k])
            S.op("act", lambda e: e.copy(hn.t[:, :, s0:s0 + n], hf.t[:]), reads=[hf.k], writes=[hn.k])
            for tt in range(4):
                ti = s0 // P + tt
                for k in range(KD):
                    S.op("pe", lambda e: e.matmul(blg.t[:, 0:NE], lhsT=hf.t[:, k, tt * P:(tt + 1) * P], rhs=self.wr.t[:, k, :],
                                                  start=(k == 0), stop=(k == KD - 1)),
                         reads=[hf.k, self.wr.k], writes=[blg.k], inc=(k == KD - 1))
                r = sm.next()
                lg, mx, nm1, sel, ex, se, den = (r.t[:, 0:8], r.t[:, 8:16], r.t[:, 16:17], r.t[:, 24:32], r.t[:, 32:40],
                                                 r.t[:, 40:48], r.t[:, 17:18])
                S.op("act", lambda e: e.copy(lg, blg.t[:, 0:NE]), reads=[blg.k], writes=[r.k])
                S.op("dve", lambda e: e.max(mx, lg), reads=[r.k], writes=[r.k])
                S.op("dve", lambda e: e.tensor_scalar(nm1, mx[:, 0:1], -1.0, None, op0=ALU.mult), reads=[r.k], writes=[r.k])
                S.op("dve", lambda e: e.tensor_scalar(sel, lg, mx[:, 1:2], None, op0=ALU.is_ge), reads=[r.k], writes=[r.k])
                S.op("act", lambda e: e.activation(ex, lg, AF.Exp, bias=nm1, scale=1.0), reads=[r.k], writes=[r.k])
                S.op("dve", lambda e: e.tensor_tensor(se, sel, ex, ALU.mult), reads=[r.k], writes=[r.k])
                S.op("dve", lambda e: e.reduce_sum(den, se, mybir.AxisListType.X), reads=[r.k], writes=[r.k])
                S.op("dve", lambda e: e.reciprocal(r.t[:, 18:19], den), reads=[r.k], writes=[r.k])
                S.op("dve", lambda e: e.tensor_scalar(comb_tok.t[:, ti, :], se, r.t[:, 18:19], None, op0=ALU.mult),
                     reads=[r.k], writes=[comb_tok.k])
        self.release_after(m2, stk + [hf.k, sm.bufs[0].k, sm.bufs[1].k])
        fb, ftk = self.ffn_bufs()
        dg = Ring([self.sb(f"dg{i}", [P, P], F32) for i in range(2)])
        elist = list(range(NE))
        if "me1" in self.skip:
            elist = [0]
        if "meA" in self.skip:
            elist = [7]
        if "meB" in self.skip:
            elist = [0, 1, 2, 3]
        if "meC" in self.skip:
            elist = [0, 1, 2, 3, 4, 5]
        tail_ = None
        for ex_ in elist:
            if "mc" in self.skip:
                continue
            for tq in range(HALF // 512):
                bk = B[tq % 2]
                for t4 in range(4):
                    ti = tq * 4 + t4
                    d_ = dg.next()
                    S.op("dve", lambda e: e.tensor_scalar(d_.t[:], self.ident_f, comb_tok.t[:, ti, ex_:ex_ + 1], None, op0=ALU.mult),
                         reads=[self.csq.k, comb_tok.k], writes=[d_.k])
                    S.op("pe", lambda e: e.matmul(bk.t[:, t4 * P:(t4 + 1) * P], lhsT=self.ones_f, rhs=d_.t[:], start=True, stop=True),
                         reads=[d_.k, self.csq.k], writes=[bk.k])
                S.op("act", lambda e: e.copy(comb.t[:, tq * 512:(tq + 1) * 512], bk.t[:]), reads=[bk.k], writes=[comb.k])
            if "mx" in self.skip:
                continue
            if tail_ is not None:
                tail_()
            tail_ = self.ffn_pass(fb, hn, HALF, 0, self.w_mg[ex_], self.w_mu[ex_], self.w_md[ex_], DFE, comb=comb, flush=False)
        if tail_ is not None:
            tail_()
        self.release_after(m, [hn.k, comb_tok.k, comb.k, dg.bufs[0].k, dg.bufs[1].k] + ftk)

    def final_phase(self, half, norm=True):
        S, B = self.S, self.B
        m = self.mark()
        ot = Ring([self.sb(f"otile{i}", [P, D], F32) for i in range(2)])
        junk = self.sb("fjunk", [P, D], BF16)
        st = Ring([self.sb(f"fst{i}", [P, 4], F32) for i in range(2)])
        fnwb = self.sb("fnwb", [P, D], F32)
        S.dma("sp", self.slot("c5"), [(fnwb.t[:], self.c_big[:, R_FNW:R_FNW + D])], writes=[fnwb.k])
        fnw = fnwb.t[:]
        self.otk = [ot.bufs[0].k, ot.bufs[1].k]
        for ti in range(HALF // P):
            c0 = ti * P
            o = ot.next()
            s4 = st.next()
            for k in range(KD):
                bk = B[k // 4]
                S.op("pe", lambda e: e.transpose(bk.t[:, (k % 4) * P:(k % 4 + 1) * P], self.xT.t[:, k, c0:c0 + P], self.ident_f),
                     reads=self.xtk([k], c0, P) + [self.csq.k], writes=[bk.k], inc=(k % 4 == 3))
            if norm:
                for hb in range(2):
                    S.op("act", lambda e: e.activation(junk.t[:, hb * 512:(hb + 1) * 512], B[hb].t[:], AF.Square,
                                                       accum_out=s4.t[:, hb:hb + 1]),
                         reads=[B[hb].k], writes=[junk.k, s4.k])
                S.op("dve", lambda e: e.tensor_tensor(s4.t[:, 2:3], s4.t[:, 0:1], s4.t[:, 1:2], ALU.add), reads=[s4.k], writes=[s4.k])
                S.op("act", lambda e: e.activation(s4.t[:, 3:4], s4.t[:, 2:3], AF.Sqrt, bias=EPS, scale=1.0 / D), reads=[s4.k], writes=[s4.k])
                S.op("dve", lambda e: e.reciprocal(s4.t[:, 0:1], s4.t[:, 3:4]), reads=[s4.k], writes=[s4.k])
                for hb in range(2):
                    S.op("dve", lambda e: e.scalar_tensor_tensor(o.t[:, hb * 512:(hb + 1) * 512], B[hb].t[:], s4.t[:, 0:1],
                                                                 fnw[:, hb * 512:(hb + 1) * 512], op0=ALU.mult, op1=ALU.mult),
                         reads=[B[hb].k, s4.k, fnwb.k], writes=[o.k])
            else:
                for hb in range(2):
                    S.op("act", lambda e: e.copy(o.t[:, hb * 512:(hb + 1) * 512], B[hb].t[:]), reads=[B[hb].k], writes=[o.k])
            r = half * HALF + c0
            S.dma("sp", self.slot("out%d" % (ti % 2)), [(self.out[r:r + P, :], o.t[:])], reads=[o.k])
        self.release_after(m, [ot.bufs[0].k, ot.bufs[1].k, junk.k, st.bufs[0].k, st.bufs[1].k, fnwb.k])

    def ssd_group(self, c0, ntok, lite, with_c=True):
        S, B = self.S, self.B
        nch = ntok // P
        m = self.mark()
        hn = self.sb("hn_s", [P, KD, ntok], BF16)
        m1 = self.mark()
        scr, stk = self.norm_scratch(256)
        for s0 in range(0, ntok, 256):
            self.norm_fm(c0 + s0, 256, C_SNW, hn, s0, scr)
        self.release_after(m1, stk)
        win = [self.sb(f"win{i}", [P, KD, 256], BF16) for i in range(2)]
        nxc = 32 if (with_c or not lite) else 24
        if "fc" in self.skip:
            nxc = 24
        xbcT = self.sb("xbcT", [P, nxc, ntok], BF16)
        cs = Ring([self.sb(f"cs{i}", [P, ntok + 3], F32) for i in range(2)])
        ca = Ring([self.sb(f"ca{i}", [P, ntok], F32) for i in range(2)])
        tks = [hn.k, xbcT.k] + [b.k for b in win + cs.bufs + ca.bufs]
        if not lite:
            zs = self.sb("zs", [P, nch, DIN], BF16)
            tks.append(zs.k)
            for cg in range(0 if "fz" in self.skip else DIN // 256):
                b = self.wctr % 2
                self.wctr += 1
                w = win[b]
                S.dma("sp", self.q_w2[b], [(w.t[:], self.w_in_b[cg].rearrange("p (k n) -> p k n", k=KD))],
                      reads=[self.winb_k[cg]], writes=[w.k])
                for ch in range(nch):
                    pb = B[ch % 2]
                    for k in range(KD):
                        S.op("pe", lambda e: e.matmul(pb.t[:, :256], lhsT=hn.t[:, k, ch * P:(ch + 1) * P], rhs=w.t[:, k, :],
                                                      start=(k == 0), stop=(k == KD - 1)),
                             reads=[hn.k, w.k], writes=[pb.k], inc=(k == KD - 1))
                    S.op("act", lambda e: e.activation(zs.t[:, ch, cg * 256:(cg + 1) * 256], pb.t[:, :256], AF.Silu),
                         reads=[pb.k], writes=[zs.k])
        wcur = [None]

        def xbc_stage1(j):
            cg, j2 = j // 2, j % 2
            if j2 == 0:
                b = self.wctr % 2
                self.wctr += 1
                wcur[0] = win[b]
                S.dma("sp", self.q_w2[b], [(wcur[0].t[:], self.w_in_b[8 + cg].rearrange("p (k n) -> p k n", k=KD))],
                      reads=[self.winb_k[8 + cg]], writes=[wcur[0].k])
            w = wcur[0]
            cb_ = cs.next()
            S.op("act", lambda e: e.copy(cb_.t[:, 0:3], self.halo.t[:, j, :]), reads=[self.halo.k], writes=[cb_.k])
            for s0 in range(0, ntok, 512):
                n = min(512, ntok - s0)
                pb = B[(j2 + s0 // 512) % 2]
                for k in range(KD):
                    S.op("pe", lambda e: e.matmul(pb.t[:, :n], lhsT=w.t[:, k, j2 * P:(j2 + 1) * P], rhs=hn.t[:, k, s0:s0 + n],
                                                  start=(k == 0), stop=(k == KD - 1)),
                         reads=[hn.k, w.k], writes=[pb.k], inc=(k == KD - 1))
                S.op("act", lambda e: e.copy(cb_.t[:, 3 + s0:3 + s0 + n], pb.t[:, :n]), reads=[pb.k], writes=[cb_.k])
            S.op("act", lambda e: e.copy(self.halo.t[:, j, :], cb_.t[:, ntok:ntok + 3]), reads=[cb_.k], writes=[self.halo.k])
            return cb_

        def xbc_stage2(j, cb_):
            a_ = ca.next()
            cw = self.cols.t[:, C_CW + 4 * j:C_CW + 4 * j + 4]
            S.op("dve", lambda e: e.tensor_scalar(a_.t[:], cb_.t[:, 0:ntok], cw[:, 0:1], self.cols.t[:, C_CB + j:C_CB + j + 1],
                                                  op0=ALU.mult, op1=ALU.add),
                 reads=[cb_.k, self.cols.k], writes=[a_.k])
            for tap in range(1, 4):
                S.op("dve", lambda e: e.scalar_tensor_tensor(a_.t[:], cb_.t[:, tap:tap + ntok], cw[:, tap:tap + 1], a_.t[:],
                                                             op0=ALU.mult, op1=ALU.add),
                     reads=[cb_.k, self.cols.k, a_.k], writes=[a_.k])
            S.op("act", lambda e: e.activation(xbcT.t[:, j, :], a_.t[:], AF.Silu), reads=[a_.k], writes=[xbcT.k])

        cb_next = xbc_stage1(0)
        for j in range(nxc):
            cb_cur = cb_next
            if j + 1 < nxc:
                cb_next = xbc_stage1(j + 1)
            xbc_stage2(j, cb_cur)
        dts = self.sb("dts", [P, nch, 6, NH], F32)
        tks.append(dts.k)
        for ch in range(nch):
            pb = B[6]
            for k in range(KD):
                S.op("pe", lambda e: e.matmul(pb.t[:, 0:NH], lhsT=hn.t[:, k, ch * P:(ch + 1) * P], rhs=self.wdt.t[:, k, :],
                                              start=(k == 0), stop=(k == KD - 1)),
                     reads=[hn.k, self.wdt.k], writes=[pb.k], inc=(k == KD - 1))
            xr, ax, ex, ln, dtv, dA = [dts.t[:, ch, i, :] for i in range(6)]
            S.op("dve", lambda e: e.tensor_tensor(xr, pb.t[:, 0:NH], self.reps.t[:, R_DTB:R_DTB + NH], ALU.add),
                 reads=[pb.k, self.reps.k], writes=[dts.k])
            S.op("dve", lambda e: e.scalar_tensor_tensor(ax, xr, -1.0, xr, op0=ALU.mult, op1=ALU.max), reads=[dts.k], writes=[dts.k])
            S.op("act", lambda e: e.activation(ex, ax, AF.Exp, scale=-1.0), reads=[dts.k], writes=[dts.k])
            S.op("act", lambda e: e.activation(ln, ex, AF.Ln, bias=1.0), reads=[dts.k], writes=[dts.k])
            S.op("dve", lambda e: e.scalar_tensor_tensor(dtv, xr, 0.0, ln, op0=ALU.max, op1=ALU.add), reads=[dts.k], writes=[dts.k])
            S.op("dve", lambda e: e.tensor_tensor(dA, dtv, self.aneg.t[:], ALU.mult), reads=[dts.k, self.aneg.k], writes=[dts.k])
        xdt = self.sb("xdt", [P, DIN], BF16)
        xw = self.sb("xw", [P, DIN], BF16)
        btok = self.sb("btok", [P, NG * P], BF16)
        sm = self.sb("ssm", [P, 6, NH], F32)
        tks += [xdt.k, xw.k, btok.k, sm.k]
        if not lite:
            xtok = self.sb("xtk", [P, DIN], BF16)
            R = Ring([self.sb(f"R{i}", [P, 4, P], F32) for i in range(4)])
            dec = Ring([self.sb(f"dec{i}", [P, 512], F32) for i in range(2)])
            wT = Ring([self.sb(f"wT{i}", [P, 4, P], BF16) for i in range(2)])
            cbm = self.sb("cbm", [P, NG, P], F32)
            yh = self.sb("yh", [P, 1024], F32)
            yg = self.sb("yg", [P, DIN], F32)
            yn = self.sb("yn", [P, DIN], BF16)
            ynT = self.sb("ynT", [P, 16, ntok], BF16)
            gst = self.sb("gst", [P, 8], F32)
            tks += [xtok.k, cbm.k, yh.k, yg.k, yn.k, ynT.k, gst.k] + [b.k for b in R.bufs + dec.bufs + wT.bufs]
        for ch in range(nch):
            tc = ch * P
            dtv, dA = dts.t[:, ch, 4, :], dts.t[:, ch, 5, :]
            acs, dif, te, eacs, etot = [sm.t[:, i, :] for i in range(5)]
            ptx = [B[2].t[:].bitcast(BF16), B[3].t[:].bitcast(BF16)]
            for j in range(16):
                S.op("pe", lambda e: e.transpose(ptx[j // 8][:, (j % 8) * P:(j % 8 + 1) * P], xbcT.t[:, j, tc:tc + P], self.identb.t[:]),
                     reads=[xbcT.k, self.identb.k], writes=[B[2 + j // 8].k], inc=(j % 8 == 7))
            for hb in range(2):
                src = ptx[hb].rearrange("p (h q) -> p h q", q=HD)
                S.op("dve", lambda e: e.tensor_tensor(xdt.t[:, hb * 1024:(hb + 1) * 1024].rearrange("p (h q) -> p h q", q=HD), src,
                                                      dtv[:, hb * 16:(hb + 1) * 16].unsqueeze(2).to_broadcast([P, 16, HD]), ALU.mult),
                     reads=[B[2 + hb].k, dts.k], writes=[xdt.k])
                if not lite and "fx" not in self.skip:
                    S.op("act", lambda e: e.copy(xtok.t[:, hb * 1024:(hb + 1) * 1024], ptx[hb]), reads=[B[2 + hb].k], writes=[xtok.k])
            ptb = B[4].t[:].bitcast(BF16)
            for g in range(NG):
                S.op("pe", lambda e: e.transpose(ptb[:, g * P:(g + 1) * P], xbcT.t[:, 16 + g, tc:tc + P], self.identb.t[:]),
                     reads=[xbcT.k, self.identb.k], writes=[B[4].k], inc=(g == NG - 1))
            S.op("act", lambda e: e.copy(btok.t[:], ptb), reads=[B[4].k], writes=[btok.k])
            pa = B[5]
            S.op("pe", lambda e: e.matmul(pa.t[:, 0:NH], lhsT=self.tri_f, rhs=dA, start=True, stop=True),
                 reads=[dts.k, self.csq.k], writes=[pa.k], inc=False)
            S.op("pe", lambda e: e.matmul(pa.t[:, NH:2 * NH], lhsT=self.ones_f, rhs=dA, start=True, stop=True),
                 reads=[dts.k, self.csq.k], writes=[pa.k])
            S.op("act", lambda e: e.copy(acs, pa.t[:, 0:NH]), reads=[pa.k], writes=[sm.k])
            S.op("dve", lambda e: e.tensor_tensor(dif, pa.t[:, NH:2 * NH], acs, ALU.subtract), reads=[pa.k, sm.k], writes=[sm.k])
            S.op("act", lambda e: e.activation(te, dif, AF.Exp), reads=[sm.k], writes=[sm.k])
            S.op("act", lambda e: e.activation(etot, pa.t[:, NH:2 * NH], AF.Exp), reads=[pa.k], writes=[sm.k])
            if not lite and "fe" not in self.skip:
                S.op("act", lambda e: e.activation(eacs, acs, AF.Exp), reads=[sm.k], writes=[sm.k])
            if not lite and "f1" not in self.skip:
                for g in range(NG):
                    bk = B[2 + g // 4]
                    S.op("pe", lambda e: e.matmul(bk.t[:, (g % 4) * P:(g % 4 + 1) * P], lhsT=xbcT.t[:, 16 + g, tc:tc + P],
                                                  rhs=xbcT.t[:, 24 + g, tc:tc + P], start=True, stop=True),
                         reads=[xbcT.k], writes=[bk.k], inc=(g % 4 == 3))
                for hb in range(2):
                    S.op("dve", lambda e: e.tensor_tensor(cbm.t[:, hb * 4:(hb + 1) * 4, :], B[2 + hb].t[:].rearrange("p (g i) -> p g i", g=4),
                                                          self.tri_f.unsqueeze(1).to_broadcast([P, 4, P]), ALU.mult),
                         reads=[B[2 + hb].k, self.csq.k], writes=[cbm.k])
                def stage_a(hq):
                    r_ = R.next()
                    S.op("pool", lambda e: e.tensor_tensor(r_.t[:], self.tri_f.unsqueeze(1).to_broadcast([P, 4, P]),
                                                           dA[:, 4 * hq:4 * hq + 4].unsqueeze(2).to_broadcast([P, 4, P]), ALU.mult),
                         reads=[self.csq.k, dts.k], writes=[r_.k])
                    pseg = B[hq % 2]
                    S.op("pe", lambda e: e.matmul(pseg.t[:], lhsT=self.lmat_f, rhs=r_.t[:].rearrange("p h i -> p (h i)"), start=True, stop=True),
                         reads=[r_.k, self.csq.k], writes=[pseg.k])
                    d_ = dec.next()
                    S.op("act", lambda e: e.activation(d_.t[:], pseg.t[:], AF.Exp), reads=[pseg.k], writes=[d_.k])
                    w_ = wT.next()
                    S.op("dve", lambda e: e.tensor_tensor(w_.t[:], d_.t[:].rearrange("p (h i) -> p h i", h=4),
                                                          cbm.t[:, hq, :].unsqueeze(1).to_broadcast([P, 4, P]), ALU.mult),
                         reads=[d_.k, cbm.k], writes=[w_.k])
                    return w_
                pyd = [B[6], B[7]]
                pyo = [B[2], B[3]]

                def a_pool(hq):
                    r_ = R.next()
                    S.op("pool", lambda e: e.tensor_tensor(r_.t[:], self.tri_f.unsqueeze(1).to_broadcast([P, 4, P]),
                                                           dA[:, 4 * hq:4 * hq + 4].unsqueeze(2).to_broadcast([P, 4, P]), ALU.mult),
                         reads=[self.csq.k, dts.k], writes=[r_.k])
                    return r_

                def a_rest(hq, r_):
                    pseg = B[hq % 2]
                    S.op("pe", lambda e: e.matmul(pseg.t[:], lhsT=self.lmat_f, rhs=r_.t[:].rearrange("p h i -> p (h i)"), start=True, stop=True),
                         reads=[r_.k, self.csq.k], writes=[pseg.k])
                    d_ = dec.next()
                    S.op("act", lambda e: e.activation(d_.t[:], pseg.t[:], AF.Exp), reads=[pseg.k], writes=[d_.k])
                    w_ = wT.next()
                    S.op("dve", lambda e: e.tensor_tensor(w_.t[:], d_.t[:].rearrange("p (h i) -> p h i", h=4),
                                                          cbm.t[:, hq, :].unsqueeze(1).to_broadcast([P, 4, P]), ALU.mult),
                         reads=[d_.k, cbm.k], writes=[w_.k])
                    return w_

                def skip_term():
                    for hh in range(2):
                        cs_ = slice(hh * 1024, (hh + 1) * 1024)
                        S.op("pool", lambda e: e.tensor_tensor(yg.t[:, cs_].rearrange("p (h q) -> p h q", q=HD),
                                                               xtok.t[:, cs_].rearrange("p (h q) -> p h q", q=HD),
                                                               self.reps.t[:, R_D + 16 * hh:R_D + 16 * hh + 16].unsqueeze(2).to_broadcast([P, 16, HD]), ALU.mult),
                             reads=[xtok.k, self.reps.k], writes=[yg.k])

                def tail_dve(hh):
                    for g in range(4 * hh, 4 * hh + 4):
                        gl = g - 4 * hh
                        bk = pyo[gl // 2]
                        S.op("pe", lambda e: e.matmul(bk.t[:, (gl % 2) * 256:(gl % 2 + 1) * 256], lhsT=xbcT.t[:, 24 + g, tc:tc + P],
                                                      rhs=self.S_b.t[:, g * 256:(g + 1) * 256], start=True, stop=True),
                             reads=[xbcT.k, self.S_b.k], writes=[bk.k], inc=(gl % 2 == 1))
                    for q in range(2):
                        hs = slice(hh * 16 + q * 8, hh * 16 + q * 8 + 8)
                        ysl = yh.t[:, q * 512:(q + 1) * 512]
                        S.op("dve", lambda e: e.tensor_tensor(ysl.rearrange("p (h q) -> p h q", q=HD), pyo[q].t[:].rearrange("p (h q) -> p h q", q=HD),
                                                              eacs[:, hs].unsqueeze(2).to_broadcast([P, 8, HD]), ALU.mult),
                             reads=[pyo[q].k, sm.k], writes=[yh.k])
                        S.op("dve", lambda e: e.tensor_tensor(ysl, ysl, pyd[q].t[:], ALU.add), reads=[pyd[q].k, yh.k], writes=[yh.k])
                    cs_ = slice(hh * 1024, (hh + 1) * 1024)
                    S.op("dve", lambda e: e.tensor_tensor(yg.t[:, cs_], yg.t[:, cs_], yh.t[:], ALU.add), reads=[yg.k, yh.k], writes=[yg.k])

                def tail_pool(hh):
                    cs_ = slice(hh * 1024, (hh + 1) * 1024)
                    S.op("pool", lambda e: e.tensor_tensor(yg.t[:, cs_], yg.t[:, cs_], zs.t[:, ch, cs_], ALU.mult),
                         reads=[yg.k, zs.k], writes=[yg.k])

                def tail_sq(hh):
                    cs_ = slice(hh * 1024, (hh + 1) * 1024)
                    S.op("act", lambda e: e.activation(yn.t[:, cs_], yg.t[:, cs_], AF.Square, accum_out=gst.t[:, hh:hh + 1]),
                         reads=[yg.k], writes=[yn.k, gst.k])

                rr = {0: a_pool(0)}
                w_next = a_rest(0, rr[0])
                for hq in range(8):
                    hh = hq // 4
                    w_ = w_next
                    if hq + 1 < 8:
                        if hq + 1 not in rr:
                            rr[hq + 1] = a_pool(hq + 1)
                        w_next = a_rest(hq + 1, rr[hq + 1])
                    if hq == 3:
                        skip_term()
                    for r4 in range(4):
                        h = 4 * hq + r4
                        hl = h - 16 * hh
                        bk = pyd[hl // 8]
                        S.op("pe", lambda e: e.matmul(bk.t[:, (hl % 8) * HD:(hl % 8 + 1) * HD], lhsT=w_.t[:, r4, :],
                                                      rhs=xdt.t[:, h * HD:(h + 1) * HD], start=True, stop=True),
                             reads=[w_.k, xdt.k], writes=[bk.k], inc=(r4 == 3))
                    if hq == 3:
                        tail_dve(0)
                        for q_ in (5, 6, 7):
                            rr[q_] = a_pool(q_)
                        tail_pool(0)
                    if hq == 7:
                        tail_dve(1)
                        tail_pool(1)
                        tail_sq(0)
                        tail_sq(1)
            if not lite and "f2" not in self.skip:
                S.op("dve", lambda e: e.tensor_tensor(gst.t[:, 2:3], gst.t[:, 0:1], gst.t[:, 1:2], ALU.add), reads=[gst.k], writes=[gst.k])
                S.op("act", lambda e: e.activation(gst.t[:, 3:4], gst.t[:, 2:3], AF.Sqrt, bias=SSM_EPS, scale=1.0 / DIN), reads=[gst.k], writes=[gst.k])
                S.op("dve", lambda e: e.reciprocal(gst.t[:, 4:5], gst.t[:, 3:4]), reads=[gst.k], writes=[gst.k])
                S.op("dve", lambda e: e.tensor_scalar(yn.t[:], yg.t[:], gst.t[:, 4:5], None, op0=ALU.mult), reads=[yg.k, gst.k], writes=[yn.k])
                pyt = [B[2].t[:].bitcast(BF16), B[3].t[:].bitcast(BF16)]
                for j in range(16):
                    S.op("pe", lambda e: e.transpose(pyt[j // 8][:, (j % 8) * P:(j % 8 + 1) * P], yn.t[:, j * P:(j + 1) * P], self.identb.t[:]),
                         reads=[yn.k, self.identb.k], writes=[B[2 + j // 8].k], inc=(j % 8 == 7))
                for hb in range(2):
                    S.op("dve", lambda e: e.tensor_tensor(ynT.t[:, hb * 8:(hb + 1) * 8, tc:tc + P], pyt[hb].rearrange("p (j t) -> p j t", j=8),
                                                          self.cols.t[:, C_GNW + hb * 8:C_GNW + hb * 8 + 8].unsqueeze(2).to_broadcast([P, 8, P]), ALU.mult),
                         reads=[B[2 + hb].k, self.cols.k], writes=[ynT.k])
            S.op("pool", lambda e: e.tensor_tensor(xw.t[:].rearrange("p (h q) -> p h q", q=HD),
                                                   xdt.t[:].rearrange("p (h q) -> p h q", q=HD),
                                                   te.unsqueeze(2).to_broadcast([P, NH, HD]), ALU.mult),
                 reads=[xdt.k, sm.k], writes=[xw.k])
            for hh in range(2):
                pst = [B[6], B[7]]
                for g in range(4 * hh, 4 * hh + 4):
                    gl = g - 4 * hh
                    bk = pst[gl // 2]
                    S.op("pe", lambda e: e.matmul(bk.t[:, (gl % 2) * 256:(gl % 2 + 1) * 256], lhsT=btok.t[:, g * P:(g + 1) * P],
                                                  rhs=xw.t[:, g * 256:(g + 1) * 256], start=True, stop=True),
                         reads=[btok.k, xw.k], writes=[bk.k], inc=(gl % 2 == 1))
                cs_ = slice(hh * 1024, (hh + 1) * 1024)
                sf = self.S_f.t[:, cs_]
                S.op("pool", lambda e: e.tensor_tensor(sf.rearrange("p (h q) -> p h q", q=HD), sf.rearrange("p (h q) -> p h q", q=HD),
                                                       etot[:, hh * 16:(hh + 1) * 16].unsqueeze(2).to_broadcast([P, 16, HD]), ALU.mult),
                     reads=[self.S_f.k, sm.k], writes=[self.S_f.k])
                for q in range(2):
                    sq_ = self.S_f.t[:, hh * 1024 + q * 512:hh * 1024 + (q + 1) * 512]
                    S.op("dve", lambda e: e.tensor_tensor(sq_, sq_, pst[q].t[:], ALU.add), reads=[pst[q].k, self.S_f.k], writes=[self.S_f.k])
                if not lite and "fs" not in self.skip:
                    S.op("dve", lambda e: e.tensor_copy(self.S_b.t[:, cs_], sf), reads=[self.S_f.k], writes=[self.S_b.k])
        if not lite and "f3" not in self.skip:
            wo = [self.sb(f"wo{i}", [P, 16, P], BF16) for i in range(2)]
            tks += [b.k for b in wo]
            for dq in range(KD):
                b = self.wctr % 2
                self.wctr += 1
                w = wo[b]
                S.dma("sp", self.q_wo[b], [(w.t[:], self.w_out_b[dq].rearrange("p (j d) -> p j d", j=16))],
                      reads=[self.woutb_k[dq]], writes=[w.k])
                pb = B[dq % 2]
                for j in range(16):
                    S.op("pe", lambda e: e.matmul(pb.t[:, :ntok], lhsT=w.t[:, j, :], rhs=ynT.t[:, j, :], start=(j == 0), stop=(j == 15)),
                         reads=[w.k, ynT.k], writes=[pb.k], inc=(j == 15))
                xs_ = self.xT.t[:, dq, c0:c0 + ntok]
                S.op("dve", lambda e: e.tensor_tensor(xs_, xs_, pb.t[:, :ntok], ALU.add), reads=[pb.k] + self.xtk([dq], c0, ntok), writes=self.xtk([dq], c0, ntok))
        self.release_after(m, tks)


def _const_tables():
    i = np.arange(P)
    ident = np.eye(P, dtype=np.float32)
    tri = (i[:, None] <= i[None, :]).astype(np.float32)
    lmat = (i[:, None] > i[None, :]).astype(np.float32)
    ones = np.ones((P, P), np.float32)
    onesmean = np.full((P, P), 1.0 / D, np.float32)
    return np.stack([ident, tri, lmat, ones, onesmean])


def _bands(special):
    s = np.arange(P)[:, None]
    t = np.arange(P)[None, :]
    cur = np.zeros((4, P, P), np.float32)
    prev = np.zeros((4, P, P), np.float32)
    for g, w in enumerate(POOL_WINDOWS):
        cnt = np.minimum(t + 1, w).astype(np.float32) if special else np.full((1, P), float(w), np.float32)
        inwin = ((s <= t) & (s > t - w)).astype(np.float32)
        cur[g] = inwin / cnt - (s == t).astype(np.float32)
        prev[g] = ((s - P) > (t - w)).astype(np.float32) / cnt
    return cur, prev


_CACHE = {}


def _get_nc(stop=None):
    if stop not in _CACHE:
        _CACHE[stop] = Builder(stop).build()
    return _CACHE[stop]


def kernel(x, pool_norm_w, pool_w, pool_scale, dense_norm_w, dense_w_gate, dense_w_up, dense_w_down,
           ssd_norm_w, ssd_w_in, ssd_conv_w, ssd_conv_b, ssd_dt_bias, ssd_a_log, ssd_d, ssd_gate_norm_w,
           ssd_w_out, moe_norm_w, moe_w_router, moe_w_gate, moe_w_up, moe_w_down, final_norm_w, _stop=None):
    f = lambda a: np.ascontiguousarray(np.asarray(a, dtype=np.float32))
    x = f(x)
    n_cores = 8
    def colmajor(v, nk):
        return f(v).reshape(nk, P).T
    cols = np.zeros((P, NCOLS), np.float32)
    cols[:, C_DNW:C_DNW + 8] = colmajor(dense_norm_w[0], 8)
    cols[:, C_SNW:C_SNW + 8] = colmajor(ssd_norm_w[0], 8)
    cols[:, C_MNW:C_MNW + 8] = colmajor(moe_norm_w[0], 8)
    cw = f(ssd_conv_w[0])
    cols[:, C_CW:C_CW + 128] = cw.reshape(4, 32, P).transpose(2, 1, 0).reshape(P, 128)
    cols[:, C_CB:C_CB + 32] = colmajor(ssd_conv_b[0], 32)
    cols[:, C_GNW:C_GNW + 16] = colmajor(ssd_gate_norm_w[0], 16)
    reps = np.zeros((P, NREPS), np.float32)
    big = np.zeros((P, 3 * D), np.float32)
    big[:, R_PNW:R_PNW + D] = f(pool_norm_w[0])[None, :]
    big[:, R_FNW:R_FNW + D] = f(final_norm_w)[None, :]
    big[:, R_PSC:R_PSC + D] = f(pool_scale[0])[None, :]
    reps[:, R_DTB:R_DTB + NH] = f(ssd_dt_bias[0])[None, :]
    reps[:, R_ALOG:R_ALOG + NH] = f(ssd_a_log[0])[None, :]
    reps[:, R_D:R_D + NH] = f(ssd_d[0])[None, :]
    csq = _const_tables()
    gcur, gprev = _bands(False)
    scur, _ = _bands(True)
    shared = {
        "c_sq": csq, "c_reps": reps, "c_big": big, "pool_w": f(pool_w[0]),
        "dense_w_gate": f(dense_w_gate[0]), "dense_w_up": f(dense_w_up[0]), "dense_w_down": f(dense_w_down[0]),
        "ssd_w_in": f(ssd_w_in[0]), "ssd_w_out": f(ssd_w_out[0]), "moe_w_router": f(moe_w_router[0]),
        "moe_w_gate": f(moe_w_gate[0]), "moe_w_up": f(moe_w_up[0]), "moe_w_down": f(moe_w_down[0]),
    }
    if _stop is not None and _stop.split(",")[0] not in ("", "nopre", "two", "two1"):
        for k_ in ("moe_w_gate", "moe_w_up", "moe_w_down"):
            del shared[k_]
    in_maps = []
    for c in range(n_cores):
        b, h = c // 2, c % 2
        own = x[b, h * NTOK:(h + 1) * NTOK]
        pre = x[b, 0:NPRE] if h == 1 else np.zeros((NPRE, D), np.float32)
        cc = cols.copy()
        cc[:, C_FLAG] = float(h)
        band = np.stack([gcur, gprev, scur if h == 1 else gcur, scur if h == 0 else gcur])
        m = dict(shared)
        m["xs"] = np.ascontiguousarray(np.concatenate([pre, own], axis=0))
        m["c_cols"] = cc
        m["c_band"] = np.ascontiguousarray(band)
        in_maps.append(m)
    if _stop in ("two", "two1"):
        cores = list(range(n_cores)) if _stop == "two" else [1]
        ncA = _get_nc("ssd")
        ncB = _get_nc("moe2")
        keysA = ("c_sq", "c_reps", "c_big", "pool_w", "dense_w_gate", "dense_w_up", "dense_w_down", "ssd_w_in",
                 "ssd_w_out", "moe_w_router", "xs", "c_cols", "c_band")
        keysB = ("c_sq", "c_reps", "c_big", "moe_w_router", "moe_w_gate", "moe_w_up", "moe_w_down", "c_cols")
        resA = run_bass_kernel_spmd(ncA, [{k_: in_maps[c][k_] for k_ in keysA} for c in cores], core_ids=list(range(len(cores))))
        mapsB = []
        for i, c in enumerate(cores):
            mb = {k_: in_maps[c][k_] for k_ in keysB}
            mb["xs"] = np.ascontiguousarray(resA.results[i]["out"])
            mapsB.append(mb)
        res = run_bass_kernel_spmd(ncB, mapsB, core_ids=list(range(len(cores))))
        if _stop == "two1":
            return res.results[0]["out"]
        out = np.empty((4, 8192, D), np.float32)
        for c in range(n_cores):
            b, h = c // 2, c % 2
            out[b, h * NTOK:(h + 1) * NTOK] = res.results[c]["out"]
        return out
    nc = _get_nc(_stop)
    if _stop is not None and "one" in _stop:
        res1 = run_bass_kernel_spmd(nc, in_maps[1:2], core_ids=[0], trace=("trace" in _stop))
        if "trace" in _stop:
            print("EXEC_TIME_NS", res1.exec_time_ns, flush=True)
        return res1.results[0]["out"]
    res = run_bass_kernel_spmd(nc, in_maps, core_ids=list(range(n_cores)))
    out = np.empty((4, 8192, D), np.float32)
    for c in range(n_cores):
        b, h = c // 2, c % 2
        out[b, h * NTOK:(h + 1) * NTOK] = res.results[c]["out"]
    return out
```
